# Optimizing a Trainium2 kernel written in Bass

```python
import jax, jax.numpy as jnp
from jax import lax
import numpy as np

D_MODEL = 1024
BATCH = 16
SEQ = 4096
DEPTH = 4

N_A_LAYERS = DEPTH // 2
N_B_LAYERS = DEPTH - N_A_LAYERS
MLSTM_HEADS = 8
MLSTM_QK_DIM = D_MODEL // 2 // MLSTM_HEADS
MLSTM_V_DIM = D_MODEL // MLSTM_HEADS
MLSTM_CHUNK = 64
MLSTM_SPLITS = [MLSTM_HEADS * MLSTM_QK_DIM, MLSTM_HEADS * MLSTM_QK_DIM, MLSTM_HEADS * MLSTM_V_DIM,
                MLSTM_HEADS * MLSTM_V_DIM, MLSTM_HEADS, MLSTM_HEADS]
MLSTM_IN = sum(MLSTM_SPLITS)
FOX_HEADS = 16
FOX_HEAD_DIM = D_MODEL // FOX_HEADS
FOX_BLOCK = 128
D_FF = 2816
CONV_WIDTH = 3
EPS = 1e-6

kernel_name = 'hybrid_mlstm_fox_yoco_trunk'


def rmsnorm(x, w):
    xf = x.astype(jnp.float32)
    y = xf * lax.rsqrt(jnp.mean(xf * xf, axis=-1, keepdims=True) + EPS)
    return (y * w.astype(jnp.float32)).astype(x.dtype)


def mlstm_mixer(h, w_in, b_gate, w_head_norm, w_out):
    B, S, _ = h.shape
    H, DK, DV, L = MLSTM_HEADS, MLSTM_QK_DIM, MLSTM_V_DIM, MLSTM_CHUNK
    NC = S // L
    proj = h @ w_in
    idx = list(np.cumsum(MLSTM_SPLITS)[:-1])
    q, k, v, o, ig, fg = jnp.split(proj, idx, axis=-1)
    f32 = jnp.float32
    ig = ig.astype(f32) + b_gate[:H].astype(f32)
    lf = jax.nn.log_sigmoid(fg.astype(f32) + b_gate[H:].astype(f32))

    def to_chunks(t, d):
        return t.astype(f32).reshape(B, NC, L, H, d).transpose(1, 0, 3, 2, 4)

    qc = to_chunks(q, DK) * (DK ** -0.5)
    kc = to_chunks(k, DK)
    vc = to_chunks(v, DV)
    ic = ig.reshape(B, NC, L, H).transpose(1, 0, 3, 2)
    fc = lf.reshape(B, NC, L, H).transpose(1, 0, 3, 2)
    causal = jnp.tril(jnp.ones((L, L), dtype=bool))

    def step(carry, inp):
        C, n, m = carry
        qt, kt, vt, it, ft = inp
        b = jnp.cumsum(ft, axis=-1)
        dmat = jnp.where(causal, b[..., :, None] - b[..., None, :] + it[..., None, :], -jnp.inf)
        m_inter = b + m[..., None]
        m_t = jnp.maximum(m_inter, jnp.max(dmat, axis=-1))
        s = jnp.einsum('bhtd,bhsd->bhts', qt, kt) * jnp.exp(dmat - m_t[..., None])
        w_inter = jnp.exp(m_inter - m_t)
        num = jnp.einsum('bhts,bhse->bhte', s, vt) + w_inter[..., None] * jnp.einsum('bhtd,bhde->bhte', qt, C)
        den = jnp.sum(s, axis=-1) + w_inter * jnp.einsum('bhtd,bhd->bht', qt, n)
        ht = num / jnp.maximum(jnp.abs(den), jnp.exp(-m_t))[..., None]
        b_last = b[..., -1]
        g = b_last[..., None] - b + it
        m_new = jnp.maximum(b_last + m, jnp.max(g, axis=-1))
        decay = jnp.exp(b_last + m - m_new)
        wk = jnp.exp(g - m_new[..., None])
        C_new = decay[..., None, None] * C + jnp.einsum('bhs,bhsd,bhse->bhde', wk, kt, vt)
        n_new = decay[..., None] * n + jnp.einsum('bhs,bhsd->bhd', wk, kt)
        return (C_new, n_new, m_new), ht

    init = (jnp.zeros((B, H, DK, DV), f32), jnp.zeros((B, H, DK), f32), jnp.zeros((B, H), f32))
    _, hs = lax.scan(step, init, (qc, kc, vc, ic, fc))
    hs = hs.transpose(1, 0, 3, 2, 4).reshape(B, S, H, DV)
    hs = hs * lax.rsqrt(jnp.mean(hs * hs, axis=-1, keepdims=True) + EPS)
    hs = hs.reshape(B, S, H * DV) * w_head_norm.astype(f32)
    hs = hs * jax.nn.sigmoid(o.astype(f32))
    return hs.astype(h.dtype) @ w_out


def shared_kv(x, kv_norm, kv_w, kv_b_f):
    B, S, _ = x.shape
    h = rmsnorm(x, kv_norm)
    proj = h @ kv_w
    k, v, f = jnp.split(proj, [D_MODEL, 2 * D_MODEL], axis=-1)
    k = k.reshape(B, S, FOX_HEADS, FOX_HEAD_DIM)
    v = v.reshape(B, S, FOX_HEADS, FOX_HEAD_DIM)
    log_f = jax.nn.log_sigmoid(f.astype(jnp.float32) + kv_b_f.astype(jnp.float32))
    c = jnp.cumsum(log_f, axis=1).transpose(0, 2, 1)
    return k, v, c


def fox_mixer(h, w_qg, w_out, k, v, c):
    B, S, _ = h.shape
    H, DH, BLK = FOX_HEADS, FOX_HEAD_DIM, FOX_BLOCK
    NQ = S // BLK
    proj = h @ w_qg
    q, g = jnp.split(proj, [D_MODEL], axis=-1)
    q = q.reshape(B, S, H, DH) * (DH ** -0.5)
    qb = q.reshape(B, NQ, BLK, H, DH).transpose(1, 0, 2, 3, 4)
    cb = c.reshape(B, H, NQ, BLK).transpose(2, 0, 1, 3)
    starts = jnp.arange(NQ) * BLK
    key_pos = jnp.arange(S)

    def block(args):
        qi, ci, st = args
        logits = jnp.einsum('bqhd,bkhd->bhqk', qi, k).astype(jnp.float32)
        logits = logits + (ci[..., :, None] - c[..., None, :])
        qpos = st + jnp.arange(BLK)
        mask = key_pos[None, :] <= qpos[:, None]
        logits = jnp.where(mask, logits, -jnp.inf)
        p = jax.nn.softmax(logits, axis=-1)
        return jnp.einsum('bhqk,bkhd->bqhd', p.astype(v.dtype), v)

    o = lax.map(block, (qb, cb, starts))
    o = o.transpose(1, 0, 2, 3, 4).reshape(B, S, D_MODEL)
    o = o * jax.nn.sigmoid(g)
    return o @ w_out


def conv_ffn(h, w_up, conv_w, conv_b, w_down):
    S = h.shape[1]
    u = h @ w_up
    up = jnp.pad(u, ((0, 0), (CONV_WIDTH - 1, 0), (0, 0)))
    y = conv_b
    for j in range(CONV_WIDTH):
        y = y + conv_w[j] * up[:, j:j + S]
    gate, val = jnp.split(y, 2, axis=-1)
    return (jax.nn.gelu(gate, approximate=True) * val) @ w_down


def setup_inputs(seed: int = 0) -> dict:
    key = jax.random.key(seed)
    ks = jax.random.split(key, 24)
    D, H = D_MODEL, MLSTM_HEADS
    f32 = jnp.float32

    def nrm(k, shape, scale):
        return jax.random.normal(k, shape, f32) * scale

    def gain(k, shape):
        return 1.0 + 0.05 * jax.random.normal(k, shape, f32)

    x = jax.random.normal(ks[0], (BATCH, SEQ, D), f32)
    norm_mix_pre = gain(ks[1], (DEPTH, D))
    norm_mix_post = gain(ks[2], (DEPTH, D))
    norm_ffn_pre = gain(ks[3], (DEPTH, D))
    norm_ffn_post = gain(ks[4], (DEPTH, D))
    mlstm_w_in = nrm(ks[5], (N_A_LAYERS, D, MLSTM_IN), D ** -0.5)
    mlstm_b_gate = jnp.concatenate([
        nrm(ks[6], (N_A_LAYERS, H), 0.1),
        jax.random.uniform(ks[7], (N_A_LAYERS, H), f32, minval=3.0, maxval=6.0)], axis=-1)
    mlstm_norm = gain(ks[8], (N_A_LAYERS, H * MLSTM_V_DIM))
    mlstm_w_out = nrm(ks[9], (N_A_LAYERS, H * MLSTM_V_DIM, D), (H * MLSTM_V_DIM) ** -0.5)
    kv_norm = gain(ks[10], (D,))
    kv_w = nrm(ks[11], (D, 2 * D + FOX_HEADS), D ** -0.5)
    kv_b_f = jax.random.uniform(ks[12], (FOX_HEADS,), f32, minval=1.0, maxval=6.0)
    fox_w_qg = nrm(ks[13], (N_B_LAYERS, D, 2 * D), D ** -0.5)
    fox_w_out = nrm(ks[14], (N_B_LAYERS, D, D), D ** -0.5)
    ffn_w_up = nrm(ks[15], (DEPTH, D, 2 * D_FF), D ** -0.5)
    ffn_conv_w = nrm(ks[16], (DEPTH, CONV_WIDTH, 2 * D_FF), CONV_WIDTH ** -0.5)
    ffn_conv_b = nrm(ks[17], (DEPTH, 2 * D_FF), 0.02)
    ffn_w_down = nrm(ks[18], (DEPTH, D_FF, D), D_FF ** -0.5)
    return {'x': x, 'norm_mix_pre': norm_mix_pre, 'norm_mix_post': norm_mix_post,
            'norm_ffn_pre': norm_ffn_pre, 'norm_ffn_post': norm_ffn_post,
            'mlstm_w_in': mlstm_w_in, 'mlstm_b_gate': mlstm_b_gate, 'mlstm_norm': mlstm_norm,
            'mlstm_w_out': mlstm_w_out, 'kv_norm': kv_norm, 'kv_w': kv_w, 'kv_b_f': kv_b_f,
            'fox_w_qg': fox_w_qg, 'fox_w_out': fox_w_out, 'ffn_w_up': ffn_w_up,
            'ffn_conv_w': ffn_conv_w, 'ffn_conv_b': ffn_conv_b, 'ffn_w_down': ffn_w_down}


def reference(x, norm_mix_pre, norm_mix_post, norm_ffn_pre, norm_ffn_post, mlstm_w_in, mlstm_b_gate,
              mlstm_norm, mlstm_w_out, kv_norm, kv_w, kv_b_f, fox_w_qg, fox_w_out, ffn_w_up,
              ffn_conv_w, ffn_conv_b, ffn_w_down):
    k_sh = v_sh = c_sh = None
    for layer in range(DEPTH):
        h = rmsnorm(x, norm_mix_pre[layer])
        if layer < N_A_LAYERS:
            y = mlstm_mixer(h, mlstm_w_in[layer], mlstm_b_gate[layer], mlstm_norm[layer], mlstm_w_out[layer])
        else:
            j = layer - N_A_LAYERS
            y = fox_mixer(h, fox_w_qg[j], fox_w_out[j], k_sh, v_sh, c_sh)
        x = x + rmsnorm(y, norm_mix_post[layer])
        h = rmsnorm(x, norm_ffn_pre[layer])
        y = conv_ffn(h, ffn_w_up[layer], ffn_conv_w[layer], ffn_conv_b[layer], ffn_w_down[layer])
        x = x + rmsnorm(y, norm_ffn_post[layer])
        if layer == N_A_LAYERS - 1:
            k_sh, v_sh, c_sh = shared_kv(x, kv_norm, kv_w, kv_b_f)
    return x
```

```python
from contextlib import ExitStack
import numpy as np
import ml_dtypes
import concourse.bass as bass
import concourse.mybir as mybir
from concourse.bass_utils import run_bass_kernel_spmd

F32 = mybir.dt.float32
BF16 = mybir.dt.bfloat16
AF = mybir.ActivationFunctionType
ALU = mybir.AluOpType
AX = mybir.AxisListType

D = 1024
KC = 8
T = 512
NCH = T // 128
DFF = 2816
MFF = 44
EPS = 1e-6
NLAYER = 4
MIN = 3088
NEG = -30000.0
DEBUG = False


class Buf:
    __slots__ = ("name", "w", "r", "excl")

    def __init__(self, name, excl=False):
        self.name = name
        self.w = None
        self.r = {}
        self.excl = excl


class SemPool:
    def __init__(self, sems):
        self.sems = sems
        self.tot = [0] * len(sems)
        self.i = 0

    def next(self):
        i = self.i
        self.i = (i + 1) % len(self.sems)
        return i


class Prog:
    def __init__(self, nc, es):
        self.nc = nc
        self.eng = {"pe": nc.tensor, "act": nc.scalar, "dve": nc.vector, "pool": nc.gpsimd, "sp": nc.sync}
        self.sem = {}
        self.cnt = {}
        self.seen = {}
        self.semobj = {}
        for e in self.eng:
            s = es.enter_context(nc.semaphore("sem_" + e))
            self.semobj["E" + e] = s
            self.cnt[e] = 0
            self.seen[e] = {}
        self.es = es
        self.npool = 0
        self.nwait = 0

    def pool(self, n):
        sems = []
        for i in range(n):
            key = "P%d_%d" % (self.npool, i)
            s = self.es.enter_context(self.nc.semaphore(key))
            self.semobj[key] = s
            sems.append(key)
        self.npool += 1
        return SemPool(sems)

    def wait(self, e, ev):
        if ev is None:
            return
        key, val = ev
        if val <= 0:
            return
        if e == "pe" and key == "Epe":
            return
        if self.seen[e].get(key, 0) >= val:
            return
        self.eng[e].wait_ge(self.semobj[key], val)
        self.seen[e][key] = val
        self.nwait += 1

    def deps(self, e, reads, writes):
        for b in reads:
            self.wait(e, b.w)
            if b.excl:
                for k, v in b.r.items():
                    self.wait(e, (k, v))
        for b in writes:
            self.wait(e, b.w)
            for k, v in b.r.items():
                self.wait(e, (k, v))

    def mark(self, ev, reads, writes):
        for b in reads:
            b.r[ev[0]] = ev[1]
        for b in writes:
            b.w = ev
            b.r = {}

    def op(self, e, fn, reads=(), writes=()):
        self.deps(e, reads, writes)
        ins = fn(self.eng[e])
        self.cnt[e] += 1
        ins.then_inc(self.semobj["E" + e], 1)
        self.mark(("E" + e, self.cnt[e]), reads, writes)

    def mm(self, items, writes):
        ev = ("Epe", self.cnt["pe"] + 1)
        ins = None
        allr = []
        for (out, lhsT, rhs, st, sp, reads) in items:
            self.deps("pe", reads, writes)
            ins = self.nc.tensor.matmul(out, lhsT, rhs, start=st, stop=sp)
            for b in reads:
                if b not in allr:
                    allr.append(b)
        self.cnt["pe"] += 1
        ins.then_inc(self.semobj["Epe"], 1)
        self.mark(ev, allr, writes)

    def transposes(self, items, writes):
        ev = ("Epe", self.cnt["pe"] + 1)
        ins = None
        allr = []
        for (out, in_, ident, reads) in items:
            self.deps("pe", reads, writes)
            ins = self.nc.tensor.transpose(out, in_, ident)
            for b in reads:
                if b not in allr:
                    allr.append(b)
        self.cnt["pe"] += 1
        ins.then_inc(self.semobj["Epe"], 1)
        self.mark(ev, allr, writes)

    def dma(self, q, out, in_, reads, writes, pool):
        i = pool.next()
        key = pool.sems[i]
        self.wait(q, (key, pool.tot[i]))
        self.deps(q, reads, writes)
        ins = self.eng[q].dma_start(out=out, in_=in_)
        ins.then_inc(self.semobj[key], 16)
        pool.tot[i] += 16
        ev = (key, pool.tot[i])
        self.mark(ev, reads, writes)
        return ev


class Rot:
    def __init__(self, items):
        self.items = items
        self.i = 0

    def next(self):
        it = self.items[self.i]
        self.i = (self.i + 1) % len(self.items)
        return it


def build(nseq, seqlen, layers=(0, 1, 2, 3), do_kv=True, parts="mf"):
    assert seqlen % T == 0
    ntile = seqlen // T
    nblk = seqlen // 128
    ntok = nseq * seqlen
    nc = bass.Bass("TRN2", target_bir_lowering=False)
    es = ExitStack()
    P = Prog(nc, es)

    def dram_in(name, shape, dt=F32):
        return nc.dram_tensor(name, list(shape), dt, kind="ExternalInput").ap()

    xT_d = dram_in("xT", [128, KC, ntok])
    outT_d = nc.dram_tensor("outT", [128, KC, ntok], F32, kind="ExternalOutput").ap()
    norms_d = dram_in("norms", [128, 17, KC])
    w_in_d = [dram_in("w_in%d" % l, [128, KC, MIN]) for l in range(2)]
    w_mout_d = [dram_in("w_mout%d" % l, [128, KC, D]) for l in range(2)]
    bgate_d = dram_in("bgate", [128, 2, 16])
    hnorm_d = dram_in("hnorm", [128, 2, D])
    kvw_d = dram_in("kvw", [128, KC, 2064])
    kvb_d = dram_in("kvb", [128, 16])
    w_qg_d = [dram_in("w_qg%d" % l, [128, KC, 2 * D]) for l in range(2)]
    w_fout_d = [dram_in("w_fout%d" % l, [64, 16, D]) for l in range(2)]
    w_up_d = [dram_in("w_up%d" % l, [128, KC, 2 * DFF]) for l in range(4)]
    w_dn_d = [dram_in("w_dn%d" % l, [128, 22, D]) for l in range(4)]
    convw_d = dram_in("convw", [128, 4, MFF, 4])
    ident_d = dram_in("ident", [128, 128])
    tri_d = dram_in("tri", [128, 128])
    negm_d = dram_in("negm", [128, 128])
    kscr_d = nc.dram_tensor("kscr", [nseq * nblk, 65, 16, 128], BF16, kind="Internal").ap()
    vscr_d = nc.dram_tensor("vscr", [nseq * nblk, 128, 16, 65], BF16, kind="Internal").ap()

    def sb(name, shape, dt):
        return es.enter_context(nc.sbuf_tensor(name, list(shape), dt))

    xT = sb("xT_s", [128, KC, T], F32); xT_bs = [Buf("xT%d" % i) for i in range(KC)]
    hT = sb("hT_s", [128, KC, T], BF16); hT_bs = [Buf("hT%d" % i) for i in range(KC)]
    yT = sb("yT_s", [128, KC, T], F32); yT_b = Buf("yT")
    sq = sb("sq_s", [128, KC, T], BF16); sq_bs = [Buf("sq%d" % i) for i in range(KC)]
    rstd = sb("rstd_s", [128, T], F32); rstd_b = Buf("rstd")
    lnt = rstd; lnt_b = rstd_b
    norms = sb("norms_s", [128, 17, KC], F32); norms_b = Buf("norms")
    gT = sb("gT_s", [128, 22, T], BF16); gT_bs = [Buf("gT%d" % i) for i in range(22)]
    convw = sb("convw_s", [128, 4, MFF, 4], F32); convw_b = Buf("convw")
    tails = sb("tails_s", [128, 4, MFF, 2], F32); tails_b = [[Buf("tail%d_%d" % (l, m)) for m in range(MFF)] for l in range(4)]
    NUE = 4
    ue = sb("ue_s", [128, NUE, T + 2], F32); ue_r = Rot([(ue[:, i, :], Buf("ue%d" % i)) for i in range(NUE)])
    yc = sb("yc_s", [128, NUE, T], F32); yc_r = Rot([(yc[:, i, :], Buf("yc%d" % i)) for i in range(NUE)])
    tmpn = yc; tmpn_bs = [yc_r.items[0][1], yc_r.items[1][1]]
    NWB = 3
    wb = sb("wb_s", [128, NWB, 4096], BF16); wb_r = Rot([(wb[:, i, :], Buf("wb%d" % i)) for i in range(NWB)])
    wsm = sb("wsm_s", [128, 2, KC, 16], BF16); wsm_r = Rot([(wsm[:, i], Buf("wsm%d" % i)) for i in range(2)])
    ident = sb("ident_s", [128, 128], BF16); ident_b = Buf("ident")
    trif = sb("trif_s", [128, 128], F32); onesf = sb("onesf_s", [128, 128], F32)
    onesb = sb("onesb_s", [128, 128], BF16)
    mask4 = sb("mask4_s", [128, 4, 128], BF16)
    negm = sb("negm_s", [128, 128], BF16)
    const_b = Buf("consts")
    qTf = sb("qT_s", [65, 8, T], BF16); qT = qTf[0:64]; qT_b = Buf("qT")
    kTmf = sb("kTm_s", [65, 8, T], BF16); kTm = kTmf[0:64]; kTm_b = Buf("kTm")
    ktok = sb("ktok_s", [128, NCH, 512], BF16); ktok_b = [Buf("ktok%d" % c) for c in range(NCH)]
    vtok = sb("vtok_s", [128, NCH, D], BF16); vtok_b = [Buf("vtok%d" % c) for c in range(NCH)]
    sotok = sb("sotok_s", [128, NCH, D], BF16); sotok_b = [Buf("sotok%d" % c) for c in range(NCH)]
    bgate = sb("bgate_s", [128, 2, 16], F32)
    hnorm = sb("hnorm_s", [128, 2, D], F32)
    Cst = sb("Cst_s", [64, 2, 8, 129], F32); Cst_b = [[Buf("C%d_%d" % (l, h)) for h in range(8)] for l in range(2)]
    Cw = sb("Cw_s", [64, 8, 129], BF16); Cw_b = [Buf("Cw%d" % h) for h in range(8)]
    Est = sb("Est_s", [128, 2, 8], F32); Est_b = [Buf("E%d" % l) for l in range(2)]
    gsm = sb("gsm_s", [128, 16, 16], F32); gsm_b = Buf("gsm"); gW_b = Buf("gW"); gOut_b = Buf("gOut")
    vext = sb("vext_s", [128, 1, 8, 129], BF16); vext_r = Rot([(vext[:, i], [Buf("vext%d_%d" % (i, h)) for h in range(8)]) for i in range(1)])
    stm = sb("stm_s", [128, 2, 4, 128], BF16); stm_r = Rot([(stm[:, i], Buf("stm%d" % i)) for i in range(2)])
    gw = sb("gw_s", [128, D], F32); gw_b = Buf("gw")
    hstok = sb("hstok_s", [128, D], BF16); hstok_b = Buf("hstok")
    junk = sb("junk_s", [128, 128], F32); junk_b = Buf("junk")
    y16 = yT[:].bitcast(BF16).rearrange("p k (a t) -> p (k a) t", t=T)
    print("y16", y16.shape)
    kTs = y16[0:65]; kTs_b = yT_b
    vts = sb("vts_s", [128, NCH, 16, 65], BF16); vts_b = Buf("vts")
    kvb = sb("kvb_s", [128, 16], F32)
    negc = sb("negc_s", [128, nblk, 16], F32); negc_b = Buf("negc")
    ncprev = sb("ncprev_s", [128, 16], F32); ncprev_b = Buf("ncprev")
    Caug = sb("Caug_s", [128, 16, 65], BF16); Caug_b = Buf("Caug")
    QTa = y16[0:65]; QTa_b = yT_b; QTrow_b = yT_b
    oT = gT[0:64, 0:16, :]; oT_bs = gT_bs[0:16]
    NST = 1
    NPT = 2
    pT = sb("pT_s", [128, NPT, T], BF16); pT_r = Rot([(pT[:, i, :], Buf("pT%d" % i)) for i in range(NPT)])
    rden = sb("rden_s", [65, 1, T], F32); rden_b = Buf("rden")
    rhl = sb("rhl_s", [65, 2, T], BF16); rhl_b = Buf("rhl")
    numsb = sb("numsb_s", [64, T], F32); numsb_b = Buf("numsb")

    NPS = 7
    ps_r = Rot([])
    for i in range(NPS):
        pt = es.enter_context(nc.psum_tensor("ps%d" % i, [128, 512], F32))
        ps_r.items.append((pt, Buf("ps%d" % i, excl=True)))
    pst = es.enter_context(nc.psum_tensor("pst", [128, 1024], BF16)); pst_b = Buf("pst", excl=True)

    wpool = P.pool(8)
    cpool = P.pool(4)
    xpool = P.pool(2)
    opool = P.pool(2)
    kvwpool = P.pool(8)
    stpool = P.pool(8)

    cev = []
    cev.append(P.dma("sp", norms[:], norms_d, [], [norms_b], cpool))
    cev.append(P.dma("sp", convw[:], convw_d, [], [convw_b], cpool))
    cev.append(P.dma("sp", trif[:], tri_d, [], [const_b], cpool))
    cev.append(P.dma("sp", bgate[:], bgate_d, [], [const_b], cpool))
    cev.append(P.dma("sp", hnorm[:], hnorm_d, [], [const_b], cpool))
    cev.append(P.dma("sp", kvb[:], kvb_d, [], [const_b], cpool))
    cev.append(P.dma("pool", negm[:], negm_d, [], [const_b], cpool))
    cev.append(P.dma("pool", ident[:], ident_d, [], [ident_b], cpool))
    for e in ("pe", "act", "dve"):
        for ev in cev:
            P.wait(e, ev)
    P.op("dve", lambda v: v.memset(onesf[:], 1.0), [], [const_b])
    P.op("dve", lambda v: v.memset(onesb[:], 1.0), [], [const_b])
    for j in range(4):
        P.op("dve", lambda v, j=j: v.tensor_copy(out=mask4[:, j, :], in_=trif[:]), [], [const_b])
    P.op("dve", lambda v: v.memset(Caug[:], 0.0), [], [Caug_b])
    P.op("dve", lambda v: v.memset(vts[:], 1.0), [], [vts_b])

    prepool = P.pool(16)
    wsrc = {}

    def convert(name, src, shape):
        dst = nc.dram_tensor(name + "_bf", list(shape), BF16, kind="Internal").ap()
        rows = shape[0] * shape[1]
        s2 = src.rearrange("p k n -> (p k) n")
        d2 = dst.rearrange("p k n -> (p k) n")
        bufs = []
        R = 32
        for r0 in range(0, rows, R):
            b = Buf(name + "_c%d" % r0)
            r1 = min(rows, r0 + R)
            P.dma("pool", d2[r0:r1, :], s2[r0:r1, :], [], [b], prepool)
            bufs.append(b)
        wsrc[name] = (dst, bufs)

    for l in layers:
        if "m" in parts and l < 2:
            convert("w_in%d" % l, w_in_d[l], [128, KC, MIN])
            convert("w_mout%d" % l, w_mout_d[l], [128, KC, D])
        if "m" in parts and l >= 2:
            if "kvw" not in wsrc and do_kv:
                convert("kvw", kvw_d, [128, KC, 2064])
            convert("w_qg%d" % (l - 2), w_qg_d[l - 2], [128, KC, 2 * D])
            convert("w_fout%d" % (l - 2), w_fout_d[l - 2], [64, 16, D])
        if "f" in parts:
            convert("w_up%d" % l, w_up_d[l], [128, KC, 2 * DFF])
            convert("w_dn%d" % l, w_dn_d[l], [128, 22, D])
        if l == 1 and do_kv and "kvw" not in wsrc:
            convert("kvw", kvw_d, [128, KC, 2064])

    def WS(name):
        return wsrc[name]

    wstate = {"pending": [], "specs": [], "pos": 0, "issued": 0}

    def wspecs_for_tile():
        specs = []

        def add(tag, name, c0, n, npart, k):
            dst, bufs = WS(name)
            specs.append((tag, dst[:, :, c0:c0 + n], npart, k, n, bufs))

        def add_kv():
            for c0 in range(0, 2048, 512):
                add("kv_%d" % c0, "kvw", c0, 512, 128, KC)
            add("kvf", "kvw", 2048, 16, 128, KC)

        if do_kv and 1 not in layers:
            add_kv()
        for l in layers:
            if "m" not in parts:
                pass
            elif l < 2:
                for c0 in range(0, 3072, 512):
                    add("min%d_%d" % (l, c0), "w_in%d" % l, c0, 512, 128, KC)
                add("mif%d" % l, "w_in%d" % l, 3072, 16, 128, KC)
                for c0 in (0, 512):
                    add("mout%d_%d" % (l, c0), "w_mout%d" % l, c0, 512, 128, KC)
            else:
                j = l - 2
                for c0 in range(0, 2048, 512):
                    add("qg%d_%d" % (j, c0), "w_qg%d" % j, c0, 512, 128, KC)
                for c0 in range(0, 1024, 256):
                    add("fout%d_%d" % (j, c0), "w_fout%d" % j, c0, 256, 64, 16)
            if "f" in parts:
                for c0 in range(0, 2 * DFF, 512):
                    add("up%d_%d" % (l, c0), "w_up%d" % l, c0, 512, 128, KC)
                for c0 in range(0, D, 128):
                    add("dn%d_%d" % (l, c0), "w_dn%d" % l, c0, 128, 128, 22)
            if l == 1 and do_kv:
                add_kv()
        return specs

    tile_specs = wspecs_for_tile()
    all_specs = tile_specs * (nseq * ntile)
    LA = 2

    def w_issue():
        i = wstate["issued"]
        if i >= len(all_specs):
            return
        tag, src, npart, k, n, rbufs = all_specs[i]
        if n == 16:
            ap, b = wsm_r.next()
            dst = ap[:, :, :]
        else:
            ap, b = wb_r.next()
            dst = ap[0:npart, 0:k * n].rearrange("p (k n) -> p k n", n=n)
        P.dma("pool", dst, src, rbufs, [b], wpool)
        wstate["pending"].append((tag, dst, b))
        wstate["issued"] = i + 1

    def w_get(tag):
        while wstate["issued"] < min(len(all_specs), wstate["pos"] + LA + 1):
            w_issue()
        t, dst, b = wstate["pending"].pop(0)
        assert t == tag, (t, tag)
        wstate["pos"] += 1
        return dst, b

    def evac(i, fn_act, fn_dve, reads, writes):
        if i % 2 == 0:
            P.op("act", fn_act, reads, writes)
        else:
            P.op("dve", fn_dve, reads, writes)

    def rmsnorm_stats(src, src_bs, nidx):
        pt, pb = ps_r.next()
        for kc in range(KC):
            sb_ = src_bs[kc] if isinstance(src_bs, list) else src_bs
            P.op("act", lambda a, kc=kc: a.activation(out=sq[:, kc, :], in_=src[:, kc, :], func=AF.Square), [sb_], [sq_bs[kc]])
        P.mm([(pt[:, :], onesb[:, :], sq[:, kc, :], kc == 0, kc == KC - 1, [sq_bs[kc]]) for kc in range(KC)], [pb])
        P.op("act", lambda a: a.activation(out=lnt[:], in_=pt[:, :], func=AF.Ln, scale=1.0 / D, bias=EPS), [pb], [lnt_b])
        P.op("act", lambda a: a.activation(out=rstd[:], in_=lnt[:], func=AF.Exp, scale=-0.5), [lnt_b], [rstd_b])

    def prenorm(nidx):
        rmsnorm_stats(xT, xT_bs, nidx)
        for kc in range(KC):
            P.op("dve", lambda v, kc=kc: v.scalar_tensor_tensor(out=hT[:, kc, :], in0=xT[:, kc, :], scalar=norms[:, nidx, kc:kc + 1],
                                                             in1=rstd[:], op0=ALU.mult, op1=ALU.mult),
                 [xT_bs[kc], rstd_b, norms_b], [hT_bs[kc]])

    def postnorm_residual(nidx):
        rmsnorm_stats(yT, yT_b, nidx)
        for kc in range(KC):
            tb = tmpn_bs[kc % 2]
            P.op("dve", lambda v, kc=kc: v.tensor_tensor(out=tmpn[:, kc % 2, :], in0=yT[:, kc, :], in1=rstd[:], op=ALU.mult),
                 [yT_b, rstd_b], [tb])
            P.op("dve", lambda v, kc=kc: v.scalar_tensor_tensor(out=xT[:, kc, :], in0=tmpn[:, kc % 2, :], scalar=norms[:, nidx, kc:kc + 1],
                                                                 in1=xT[:, kc, :], op0=ALU.mult, op1=ALU.add),
                 [tb, norms_b], [xT_bs[kc]])

    def proj_fm(wdst, w_b, m_lo, m_n, src, src_b, kcn, out_fn, npart=128, mwidth=128):
        for mi in range(m_n):
            pt, pb = ps_r.next()
            c0 = m_lo + mi * mwidth
            P.mm([(pt[0:mwidth, :], wdst[0:npart, kc, c0:c0 + mwidth], src[0:npart, kc, :], kc == 0, kc == kcn - 1,
                   [w_b, (src_b[kc] if isinstance(src_b, list) else src_b)])
                  for kc in range(kcn)], [pb])
            out_fn(mi, pt, pb)

    def ffn(l):
        ev_i = [0]
        for blk in range(11):
            wdst, w_b = w_get("up%d_%d" % (l, blk * 512))
            for mi in range(4):
                m = blk * 4 + mi
                pt, pb = ps_r.next()
                P.mm([(pt[:, :], wdst[:, kc, mi * 128:(mi + 1) * 128], hT[:, kc, :], kc == 0, kc == KC - 1, [w_b, hT_bs[kc]])
                      for kc in range(KC)], [pb])
                uea, ueb = ue_r.next()
                yca, ycb = yc_r.next()
                tb = tails_b[l][m]
                P.op("dve", lambda v: v.tensor_copy(out=uea[:, 0:2], in_=tails[:, l, m, :]), [tb], [ueb])
                P.op("act", lambda a: a.copy(out=uea[:, 2:T + 2], in_=pt[:, :]), [pb], [ueb])
                P.op("dve", lambda v: v.tensor_copy(out=tails[:, l, m, :], in_=uea[:, T:T + 2]), [ueb], [tb])
                P.op("act", lambda a: a.activation(out=yca, in_=uea[:, 2:T + 2], func=AF.Identity, scale=convw[:, l, m, 2:3],
                                                   bias=convw[:, l, m, 3:4]), [ueb, convw_b], [ycb])
                P.op("dve", lambda v: v.scalar_tensor_tensor(out=yca, in0=uea[:, 1:T + 1], scalar=convw[:, l, m, 1:2], in1=yca,
                                                             op0=ALU.mult, op1=ALU.add), [ueb, convw_b, ycb], [ycb])
                P.op("dve", lambda v: v.scalar_tensor_tensor(out=yca, in0=uea[:, 0:T], scalar=convw[:, l, m, 0:1], in1=yca,
                                                             op0=ALU.mult, op1=ALU.add), [ueb, convw_b, ycb], [ycb])
                if m < 22:
                    P.op("act", lambda a: a.activation(out=gT[:, m, :], in_=yca, func=AF.Gelu_apprx_tanh), [ycb], [gT_bs[m]])
                else:
                    mm_ = m - 22
                    P.op("dve", lambda v: v.tensor_tensor(out=gT[:, mm_, :], in0=gT[:, mm_, :], in1=yca, op=ALU.mult),
                         [ycb], [gT_bs[mm_]])
        for mo in range(8):
            wdst, w_b = w_get("dn%d_%d" % (l, mo * 128))
            pt, pb = ps_r.next()
            P.mm([(pt[:, :], wdst[:, kc, :], gT[:, kc, :], kc == 0, kc == 21, [w_b, gT_bs[kc]]) for kc in range(22)], [pb])
            evac(mo, lambda a: a.copy(out=yT[:, mo, :], in_=pt[:, :]), lambda v: v.tensor_copy(out=yT[:, mo, :], in_=pt[:, :]), [pb], [yT_b])

    def mlstm(l, first_tile):
        if first_tile:
            P.op("dve", lambda v: v.memset(Cst[:, l], 0.0), [], Cst_b[l])
            P.op("dve", lambda v: v.memset(Est[:, l, :], 1.0), [], [Est_b[l]])
        wdst, w_b = w_get("min%d_%d" % (l, 0))
        proj_fm(wdst, w_b, 0, 8, hT, hT_bs, KC,
                lambda h, pt, pb: evac(h, lambda a: a.mul(out=qT[:, h, :], in_=pt[0:64, :], mul=0.125),
                                       lambda v: v.tensor_scalar(out=qT[:, h, :], in0=pt[0:64, :], scalar1=0.125, scalar2=None, op0=ALU.mult),
                                       [pb], [qT_b]), mwidth=64)
        wdst, w_b = w_get("min%d_%d" % (l, 512))
        proj_fm(wdst, w_b, 0, 8, hT, hT_bs, KC,
                lambda h, pt, pb: evac(h + 1, lambda a: a.copy(out=kTm[:, h, :], in_=pt[0:64, :]),
                                       lambda v: v.tensor_copy(out=kTm[:, h, :], in_=pt[0:64, :]), [pb], [kTm_b]), mwidth=64)
        for c in range(NCH):
            pt, pb = ps_r.next()
            P.mm([(pt[:, :], hT[:, kc, c * 128:(c + 1) * 128], wdst[:, kc, :], kc == 0, kc == KC - 1, [w_b, hT_bs[kc]]) for kc in range(KC)], [pb])
            evac(c, lambda a: a.copy(out=ktok[:, c, :], in_=pt[:, :]), lambda v: v.tensor_copy(out=ktok[:, c, :], in_=pt[:, :]), [pb], [ktok_b[c]])
        for half in range(2):
            wdst, w_b = w_get("min%d_%d" % (l, 1024 + half * 512))
            for c in range(NCH):
                pt, pb = ps_r.next()
                P.mm([(pt[:, :], hT[:, kc, c * 128:(c + 1) * 128], wdst[:, kc, :], kc == 0, kc == KC - 1, [w_b, hT_bs[kc]]) for kc in range(KC)], [pb])
                evac(c + half, lambda a: a.copy(out=vtok[:, c, half * 512:(half + 1) * 512], in_=pt[:, :]),
                     lambda v: v.tensor_copy(out=vtok[:, c, half * 512:(half + 1) * 512], in_=pt[:, :]), [pb], [vtok_b[c]])
        for half in range(2):
            wdst, w_b = w_get("min%d_%d" % (l, 2048 + half * 512))
            for c in range(NCH):
                pt, pb = ps_r.next()
                P.mm([(pt[:, :], hT[:, kc, c * 128:(c + 1) * 128], wdst[:, kc, :], kc == 0, kc == KC - 1, [w_b, hT_bs[kc]]) for kc in range(KC)], [pb])
                P.op("act", lambda a: a.activation(out=sotok[:, c, half * 512:(half + 1) * 512], in_=pt[:, :], func=AF.Sigmoid), [pb], [sotok_b[c]])
        wif, wif_b = w_get("mif%d" % l)
        G = gsm
        for c in range(NCH):
            cs = slice(c * 128, (c + 1) * 128)
            pg, pgb = ps_r.next()
            P.mm([(pg[:, 0:16], hT[:, kc, cs], wif[:, kc, :], kc == 0, kc == KC - 1, [wif_b, hT_bs[kc]]) for kc in range(KC)], [pgb])
            P.op("dve", lambda v: v.tensor_tensor(out=G[:, 0, :], in0=pg[:, 0:16], in1=bgate[:, l, :], op=ALU.add), [pgb], [gsm_b])
            P.op("act", lambda a: a.activation(out=G[:, 1, 0:8], in_=G[:, 0, 8:16], func=AF.Exp, scale=-1.0), [gsm_b], [gsm_b])
            P.op("act", lambda a: a.activation(out=G[:, 1, 0:8], in_=G[:, 1, 0:8], func=AF.Ln, bias=1.0), [gsm_b], [gsm_b])
            pc, pcb = ps_r.next()
            P.mm([(pc[:, 0:8], trif[:, :], G[:, 1, 0:8], True, True, [gsm_b]),
                  (pc[:, 8:16], onesf[:, :], G[:, 1, 0:8], True, True, [gsm_b])], [pcb])
            P.op("dve", lambda v: v.tensor_tensor(out=G[:, 2, 0:8], in0=pc[:, 0:8], in1=G[:, 0, 0:8], op=ALU.add), [pcb, gsm_b], [gsm_b])
            P.op("act", lambda a: a.activation(out=G[:, 1, 8:16], in_=G[:, 2, 0:8], func=AF.Exp), [gsm_b], [gsm_b])
            P.op("act", lambda a: a.activation(out=G[:, 5, 0:8], in_=pc[:, 8:16], func=AF.Exp, scale=-1.0), [pcb], [gsm_b])
            P.op("act", lambda a: a.activation(out=G[:, 5, 8:16], in_=pc[:, 0:8], func=AF.Exp), [pcb], [gsm_b])
            pe_, peb = ps_r.next()
            P.mm([(pe_[:, 0:8], onesf[:, :], G[:, 1, 8:16], True, True, [gsm_b])], [peb])
            P.op("dve", lambda v: v.tensor_tensor(out=G[:, 3, 0:8], in0=pe_[:, 0:8], in1=Est[:, l, :], op=ALU.add), [peb, Est_b[l]], [gsm_b])
            P.op("dve", lambda v: v.reciprocal(out=G[:, 3, 8:16], in_=G[:, 3, 0:8]), [gsm_b], [gsm_b])
            P.op("dve", lambda v: v.tensor_tensor(out=G[:, 4, 0:8], in0=G[:, 1, 8:16], in1=G[:, 3, 8:16], op=ALU.mult), [gsm_b], [gW_b])
            P.op("dve", lambda v: v.tensor_tensor(out=G[:, 4, 8:16], in0=Est[:, l, :], in1=G[:, 3, 8:16], op=ALU.mult), [gsm_b, Est_b[l], gW_b], [gW_b])
            P.op("dve", lambda v: v.tensor_tensor(out=Est[:, l, :], in0=G[:, 5, 0:8], in1=G[:, 3, 0:8], op=ALU.mult), [gsm_b], [Est_b[l]])
            P.op("dve", lambda v: v.tensor_tensor(out=G[:, 6, 0:8], in0=G[:, 5, 8:16], in1=G[:, 3, 8:16], op=ALU.mult), [gsm_b], [gsm_b])
            for h in range(8):
                P.op("act", lambda a, h=h: a.activation(out=Cw[:, h, :], in_=Cst[:, l, h, :], func=AF.Identity, scale=G[0:64, 4, 8 + h:9 + h]),
                     [Cst_b[l][h], gW_b], [Cw_b[h]])
            vxa, vxb = vext_r.next()
            for h in range(8):
                P.op("act", lambda a, h=h: a.activation(out=vxa[:, h, 0:128], in_=vtok[:, c, h * 128:(h + 1) * 128], func=AF.Identity,
                                                        scale=G[:, 4, h:h + 1]), [vtok_b[c], gW_b], [vxb[h]])
            P.op("dve", lambda v: v.tensor_copy(out=vxa[:, :, 128], in_=G[:, 4, 0:8]), [gW_b], vxb)
            sts = []
            for hg in range(2):
                pS, pSb = ps_r.next()
                P.mm([(pS[:, j * 128:(j + 1) * 128], kTm[:, hg * 4 + j, cs], qT[:, hg * 4 + j, cs], True, True, [kTm_b, qT_b]) for j in range(4)], [pSb])
                sta, stb = stm_r.next()
                P.op("dve", lambda v, sta=sta, pS=pS: v.tensor_tensor(out=sta[:, :, :], in0=pS[:, :].rearrange("p (j t) -> p j t", t=128), in1=mask4[:, :, :], op=ALU.mult),
                     [pSb, const_b], [stb])
                sts.append((sta, stb))
            nums = {}
            for grp in ((0, 1, 2), (3, 4, 5), (6, 7)):
                pn, pnb = ps_r.next()
                items = []
                for j, h in enumerate(grp):
                    sta, stb = sts[h // 4]
                    o_ = pn[:, j * 129:(j + 1) * 129]
                    items.append((o_, sta[:, h % 4, :], vxa[:, h, :], True, False, [stb, vxb[h]]))
                    items.append((o_, qT[:, h, cs], Cw[:, h, :], False, True, [qT_b, Cw_b[h]]))
                    nums[h] = (pn, pnb, j)
                P.mm(items, [pnb])
            for hg in range(3):
                grp = ((0, 1, 2), (3, 4, 5), (6, 7))[hg]
                pd, pdb = ps_r.next()
                P.mm([(pd[0:64, j * 129:(j + 1) * 129], ktok[:, c, h * 64:(h + 1) * 64], vxa[:, h, :], True, True, [ktok_b[c], vxb[h]]) for j, h in enumerate(grp)], [pdb])
                for j, h in enumerate(grp):
                    P.op("dve", lambda v, j=j, h=h, pd=pd: v.scalar_tensor_tensor(out=Cst[:, l, h, :], in0=Cst[:, l, h, :], scalar=G[0:64, 4, 8 + h:9 + h],
                                                                                   in1=pd[0:64, j * 129:(j + 1) * 129], op0=ALU.mult, op1=ALU.add),
                         [pdb, gW_b, Cw_b[h]], [Cst_b[l][h]])
            for h in range(8):
                pn, pnb, j = nums[h]
                P.op("act", lambda a, h=h, pn=pn, j=j: a.activation(out=G[:, 6, 8 + h:9 + h], in_=pn[:, j * 129 + 128:j * 129 + 129], func=AF.Abs),
                     [pnb], [gOut_b])
            P.op("dve", lambda v: v.tensor_tensor(out=G[:, 7, 0:8], in0=G[:, 6, 8:16], in1=G[:, 6, 0:8], op=ALU.max), [gsm_b, gOut_b], [gOut_b])
            P.op("dve", lambda v: v.reciprocal(out=G[:, 7, 8:16], in_=G[:, 7, 0:8]), [gOut_b], [gOut_b])
            P.op("dve", lambda v: v.memset(G[:, 8, :], 0.0), [], [gOut_b])
            for h in range(8):
                pn, pnb, j = nums[h]
                P.op("act", lambda a, h=h, pn=pn, j=j: a.activation(out=junk[:, :], in_=pn[:, j * 129:j * 129 + 128], func=AF.Square, scale=G[:, 7, 8 + h:9 + h],
                                                                    accum_out=G[:, 8, h:h + 1]), [pnb, gOut_b], [junk_b, gOut_b])
            P.op("act", lambda a: a.activation(out=G[:, 9, 0:8], in_=G[:, 8, 0:8], func=AF.Ln, scale=1.0 / 128, bias=EPS), [gOut_b], [gOut_b])
            P.op("act", lambda a: a.activation(out=G[:, 8, 8:16], in_=G[:, 9, 0:8], func=AF.Exp, scale=-0.5), [gOut_b], [gOut_b])
            P.op("dve", lambda v: v.tensor_tensor(out=G[:, 9, 8:16], in0=G[:, 8, 8:16], in1=G[:, 7, 8:16], op=ALU.mult), [gOut_b], [gOut_b])
            P.op("dve", lambda v: v.tensor_tensor(out=gw[:, :], in0=sotok[:, c, :], in1=hnorm[:, l, :], op=ALU.mult), [sotok_b[c], const_b], [gw_b])
            for h in range(8):
                pn, pnb, j = nums[h]
                P.op("dve", lambda v, h=h, pn=pn, j=j: v.scalar_tensor_tensor(out=hstok[:, h * 128:(h + 1) * 128], in0=pn[:, j * 129:j * 129 + 128],
                                                                               scalar=G[:, 9, 8 + h:9 + h], in1=gw[:, h * 128:(h + 1) * 128], op0=ALU.mult, op1=ALU.mult),
                     [pnb, gOut_b, gw_b], [hstok_b])
            P.transposes([(pst[:, h * 128:(h + 1) * 128], hstok[:, h * 128:(h + 1) * 128], ident[:, :], [hstok_b, ident_b]) for h in range(8)], [pst_b])
            P.op("act", lambda a: a.copy(out=sq[:, :, cs], in_=pst[:, :].rearrange("p (k t) -> p k t", t=128)), [pst_b], sq_bs)
        for half in range(2):
            wdst, w_b = w_get("mout%d_%d" % (l, half * 512))
            proj_fm(wdst, w_b, 0, 4, sq, sq_bs, KC,
                    lambda mi, pt, pb, half=half: evac(mi, lambda a: a.copy(out=yT[:, half * 4 + mi, :], in_=pt[:, :]),
                                                       lambda v: v.tensor_copy(out=yT[:, half * 4 + mi, :], in_=pt[:, :]), [pb], [yT_b]))

    def kv_stage(seq, ti):
        gblk0 = seq * nblk + ti * NCH
        if ti == 0:
            P.op("dve", lambda v: v.memset(ncprev[:], 0.0), [], [ncprev_b])
        prenorm(16)
        P.op("dve", lambda v: v.memset(kTs[64:65, :, :], 1.0), [], [kTs_b])
        for half in range(2):
            wdst, w_b = w_get("kv_%d" % (half * 512))
            proj_fm(wdst, w_b, 0, 8, hT, hT_bs, KC,
                    lambda h, pt, pb, half=half: evac(h, lambda a: a.copy(out=kTs[0:64, half * 8 + h, :], in_=pt[0:64, :]),
                                                      lambda v: v.tensor_copy(out=kTs[0:64, half * 8 + h, :], in_=pt[0:64, :]), [pb], [kTs_b]), mwidth=64)
        for half in range(2):
            wdst, w_b = w_get("kv_%d" % (1024 + half * 512))
            for c in range(NCH):
                pt, pb = ps_r.next()
                P.mm([(pt[:, :], hT[:, kc, c * 128:(c + 1) * 128], wdst[:, kc, :], kc == 0, kc == KC - 1, [w_b, hT_bs[kc]]) for kc in range(KC)], [pb])
                evac(c + half, lambda a: a.copy(out=vts[:, c, half * 8:(half + 1) * 8, 0:64], in_=pt[:, :].rearrange("p (h d) -> p h d", d=64)),
                     lambda v: v.tensor_copy(out=vts[:, c, half * 8:(half + 1) * 8, 0:64], in_=pt[:, :].rearrange("p (h d) -> p h d", d=64)), [pb], [vts_b])
        wf, wf_b = w_get("kvf")
        G = gsm
        for c in range(NCH):
            cs = slice(c * 128, (c + 1) * 128)
            blk = ti * NCH + c
            pg, pgb = ps_r.next()
            P.mm([(pg[:, 0:16], hT[:, kc, cs], wf[:, kc, :], kc == 0, kc == KC - 1, [wf_b, hT_bs[kc]]) for kc in range(KC)], [pgb])
            P.op("dve", lambda v: v.tensor_tensor(out=G[:, 0, :], in0=pg[:, 0:16], in1=kvb[:, :], op=ALU.add), [pgb], [gsm_b])
            P.op("act", lambda a: a.activation(out=G[:, 1, :], in_=G[:, 0, :], func=AF.Exp, scale=-1.0), [gsm_b], [gsm_b])
            P.op("act", lambda a: a.activation(out=G[:, 1, :], in_=G[:, 1, :], func=AF.Ln, bias=1.0), [gsm_b], [gsm_b])
            pc, pcb = ps_r.next()
            P.mm([(pc[:, 0:16], trif[:, :], G[:, 1, :], True, True, [gsm_b]),
                  (pc[:, 16:32], onesf[:, :], G[:, 1, :], True, True, [gsm_b])], [pcb])
            P.op("dve", lambda v: v.tensor_tensor(out=negc[:, blk, :], in0=pc[:, 0:16], in1=ncprev[:, :], op=ALU.add), [pcb, ncprev_b], [negc_b])
            P.op("dve", lambda v: v.tensor_tensor(out=ncprev[:, :], in0=pc[:, 16:32], in1=ncprev[:, :], op=ALU.add), [pcb], [ncprev_b])
        kvbufs = []
        for c in range(NCH):
            kb_ = Buf("kscr")
            vb_ = Buf("vscr")
            P.dma("sp", kscr_d[gblk0 + c], kTs[0:65, :, c * 128:(c + 1) * 128], [kTs_b], [kb_], kvwpool)
            P.dma("sp", vscr_d[gblk0 + c], vts[:, c, :, :], [vts_b], [vb_], kvwpool)
            kvbufs.append((kb_, vb_))
        return kvbufs

    def fox(j, seq, ti, kvb_all):
        for half in range(2):
            wdst, w_b = w_get("qg%d_%d" % (j, half * 512))
            proj_fm(wdst, w_b, 0, 8, hT, hT_bs, KC,
                    lambda h, pt, pb, half=half: evac(h, lambda a: a.mul(out=QTa[0:64, half * 8 + h, :], in_=pt[0:64, :], mul=0.125),
                                                      lambda v: v.tensor_scalar(out=QTa[0:64, half * 8 + h, :], in0=pt[0:64, :], scalar1=0.125, scalar2=None, op0=ALU.mult),
                                                      [pb], [QTa_b]), mwidth=64)
        for c in range(NCH):
            cs = slice(c * 128, (c + 1) * 128)
            blk = ti * NCH + c
            P.op("dve", lambda v: v.tensor_scalar(out=Caug[:, :, 64], in0=negc[:, blk, :], scalar1=-1.0, scalar2=None, op0=ALU.mult), [negc_b], [Caug_b])
            for hg in range(4):
                pr, prb = ps_r.next()
                P.mm([(pr[0:65, jj * 128:(jj + 1) * 128], Caug[:, hg * 4 + jj, :], ident[:, :], True, True, [Caug_b, ident_b]) for jj in range(4)], [prb])
                evac(hg, lambda a, pr=pr, hg=hg: a.copy(out=QTa[64:65, hg * 4:hg * 4 + 4, cs], in_=pr[64:65, :].rearrange("p (j t) -> p j t", t=128)),
                     lambda v, pr=pr, hg=hg: v.tensor_copy(out=QTa[64:65, hg * 4:hg * 4 + 4, cs], in_=pr[64:65, :].rearrange("p (j t) -> p j t", t=128)),
                     [prb], [QTrow_b])
        for half in range(2):
            wdst, w_b = w_get("qg%d_%d" % (j, 1024 + half * 512))
            proj_fm(wdst, w_b, 0, 8, hT, hT_bs, KC,
                    lambda h, pt, pb, half=half: P.op("act", lambda a: a.activation(out=oT[:, half * 8 + h, :], in_=pt[0:64, :], func=AF.Sigmoid), [pb], [oT_bs[half * 8 + h]]),
                    mwidth=64)
        nkb = ti * NCH + NCH
        GB = 8
        kslots = [(qTf[:].rearrange("p a t -> p (a t)").rearrange("p (b h k) -> p b h k", h=4, k=128), [qT_b]),
                  (kTmf[:].rearrange("p a t -> p (a t)").rearrange("p (b h k) -> p b h k", h=4, k=128), [kTm_b])]
        vslots = [(vtok[:].rearrange("p a t -> p (a t)")[:, 0:GB * 4 * 65].rearrange("p (b h d) -> p b h d", h=4, d=65), vtok_b),
                  (sotok[:].rearrange("p a t -> p (a t)")[:, 0:GB * 4 * 65].rearrange("p (b h d) -> p b h d", h=4, d=65), sotok_b)]
        ps_all = list(ps_r.items)
        slot_i = 0
        for hg in range(4):
            accs = ps_all[0:4]
            ps_r.items = ps_all[4:NPS]
            ps_r.i = 0
            for g0 in range(0, nkb, GB):
                nb = min(GB, nkb - g0)
                ksv, ksbs = kslots[slot_i % 2]
                vsv, vsbs = vslots[slot_i % 2]
                slot_i += 1
                gk0 = seq * nblk + g0
                P.dma("sp", ksv[:, 0:nb], kscr_d[gk0:gk0 + nb, :, hg * 4:hg * 4 + 4, :].rearrange("b p h k -> p b h k"),
                      [kvb_all[g0 + i][0] for i in range(nb)], ksbs, stpool)
                P.dma("sp", vsv[:, 0:nb], vscr_d[gk0:gk0 + nb, :, hg * 4:hg * 4 + 4, :].rearrange("b p h d -> p b h d"),
                      [kvb_all[g0 + i][1] for i in range(nb)], vsbs, stpool)
                pairs = [(kb, hh) for kb in range(g0, g0 + nb) for hh in range(4)]

                def emit_S(kb, hh):
                    ksa = ksv[:, kb - g0]
                    jd = kb - ti * NCH
                    q0 = max(jd, 0) * 128
                    h = hg * 4 + hh
                    pS, pSb = ps_r.next()
                    rds = ksbs + [QTa_b]
                    if jd < 0:
                        items = [(pS[:, 0:T], ksa[:, hh, :], QTa[:, h, 0:T], True, True, rds)]
                    else:
                        items = [(pS[:, q0:q0 + 128], ksa[:, hh, :], QTa[:, h, q0:q0 + 128], True, False, rds),
                                 (pS[:, q0:q0 + 128], ident[:, :], negm[:, :], False, True, [ident_b, const_b])]
                        if q0 + 128 < T:
                            items.append((pS[:, q0 + 128:T], ksa[:, hh, :], QTa[:, h, q0 + 128:T], True, True, rds))
                    P.mm(items, [pSb])
                    return pS, pSb, q0

                def emit_rest(kb, hh, pS, pSb, q0):
                    vsa = vsv[:, kb - g0]
                    h = hg * 4 + hh
                    pta, ptb = pT_r.next()
                    P.op("act", lambda a: a.activation(out=pta[:, q0:T], in_=pS[:, q0:T], func=AF.Exp, bias=negc[:, kb, h:h + 1]),
                         [pSb, negc_b], [ptb])
                    pa, pab = accs[hh]
                    P.mm([(pa[0:65, q0:T], vsa[:, hh, :], pta[:, q0:T], kb == 0, kb == nkb - 1, vsbs + [ptb])], [pab])

                cur = emit_S(*pairs[0])
                for pi in range(len(pairs)):
                    nxt = emit_S(*pairs[pi + 1]) if pi + 1 < len(pairs) else None
                    emit_rest(pairs[pi][0], pairs[pi][1], *cur)
                    cur = nxt
            for hh in range(4):
                h = hg * 4 + hh
                pa, pab = accs[hh]
                P.op("dve", lambda v, pa=pa: v.reciprocal(out=rden[64:65, 0, :], in_=pa[64:65, :]), [pab], [rden_b])
                pb_, pbb = ps_r.next()
                P.mm([(pb_[0:64, :], onesf[64:65, 0:64], rden[64:65, 0, :], True, True, [rden_b])], [pbb])
                P.op("dve", lambda v, h=h, pa=pa: v.tensor_tensor(out=numsb[:, :], in0=pa[0:64, :], in1=oT[:, h, :], op=ALU.mult), [pab, oT_bs[h]], [numsb_b])
                P.op("dve", lambda v, h=h, pb_=pb_: v.tensor_tensor(out=oT[:, h, :], in0=numsb[:, :], in1=pb_[0:64, :], op=ALU.mult), [numsb_b, pbb], [oT_bs[h]])
        ps_r.items = ps_all
        ps_r.i = 0
        for q in range(4):
            wdst, w_b = w_get("fout%d_%d" % (j, q * 256))
            proj_fm(wdst, w_b, 0, 2, oT, oT_bs, 16,
                    lambda mi, pt, pb, q=q: evac(mi, lambda a: a.copy(out=yT[:, q * 2 + mi, :], in_=pt[:, :]),
                                                 lambda v: v.tensor_copy(out=yT[:, q * 2 + mi, :], in_=pt[:, :]), [pb], [yT_b]), npart=64)

    out_evs = []
    for seq in range(nseq):
        kvb_all = []
        for ti in range(ntile):
            t0 = seq * seqlen + ti * T
            P.dma("sp", xT[:], xT_d[:, :, t0:t0 + T], [], xT_bs, xpool)
            if do_kv and 1 not in layers:
                kvb_all.extend(kv_stage(seq, ti))
            for l in layers:
                if ti == 0:
                    for m in range(MFF):
                        pass
                    P.op("dve", lambda v, l=l: v.memset(tails[:, l], 0.0), [], [tails_b[l][m] for m in range(MFF)])
                if "m" in parts:
                    prenorm(l * 4 + 0)
                    if l < 2:
                        mlstm(l, ti == 0)
                    else:
                        fox(l - 2, seq, ti, kvb_all)
                    postnorm_residual(l * 4 + 1)
                if "f" in parts:
                    prenorm(l * 4 + 2)
                    ffn(l)
                    postnorm_residual(l * 4 + 3)
                if l == 1 and do_kv:
                    kvb_all.extend(kv_stage(seq, ti))
            out_evs.append(P.dma("sp", outT_d[:, :, t0:t0 + T], xT[:], xT_bs, [], opool))
    if DEBUG == 2:
        dbg = dict(QTa=(QTa, [yT_b], [65, 16, T], BF16), negc=(negc, [negc_b], [128, nblk, 16], F32), oT=(oT, oT_bs, [64, 16, T], BF16),
                   hT=(hT, hT_bs, [128, KC, T], BF16), pT=(pT, [b for _, b in pT_r.items], [128, NPT, T], BF16), rden=(rden, [rden_b], [65, 1, T], F32),
                   numsb=(numsb, [numsb_b], [64, T], F32))
        dpool = P.pool(1)
        for name, (t_, bufs, shape, dt) in dbg.items():
            dd = nc.dram_tensor("dbg_" + name, shape, dt, kind="ExternalOutput").ap()
            out_evs.append(P.dma("sp", dd, t_[:], bufs, [], dpool))
            P.wait("sp", out_evs[-1])
    if DEBUG == 1:
        dbg = dict(qT=(qT, [qT_b], [64, 8, T], BF16), kTm=(kTm, [kTm_b], [64, 8, T], BF16), vtok=(vtok, vtok_b, [128, NCH, D], BF16),
                   sotok=(sotok, sotok_b, [128, NCH, D], BF16), hstok=(hstok, [hstok_b], [128, D], BF16), hsT=(sq, sq_bs, [128, KC, T], BF16),
                   G=(gsm, [gsm_b], [128, 16, 16], F32), Cst=(Cst, Cst_b[0] + Cst_b[1], [64, 2, 8, 129], F32), hT=(hT, hT_bs, [128, KC, T], BF16),
                   yT=(yT, [yT_b], [128, KC, T], F32), ktok=(ktok, ktok_b, [128, NCH, 512], BF16), Est=(Est, Est_b, [128, 2, 8], F32))
        dpool = P.pool(1)
        for name, (t_, bufs, shape, dt) in dbg.items():
            dd = nc.dram_tensor("dbg_" + name, shape, dt, kind="ExternalOutput").ap()
            out_evs.append(P.dma("sp", dd, t_[:], bufs, [], dpool))
            P.wait("sp", out_evs[-1])
    for ev in out_evs[-3:]:
        P.wait("sp", ev)
    assert wstate["pos"] == len(all_specs), (wstate["pos"], len(all_specs))
    build.stats = dict(cnt=dict(P.cnt), nwait=P.nwait)
    build.es = es
    return nc


def _kmajor(w, kc=None):
    K, N = w.shape
    kc = K // 128
    return np.ascontiguousarray(w.reshape(kc, 128, N).transpose(1, 0, 2))


def prep_shared(inp):
    f = np.float32
    sh = {}
    norms = np.zeros((17, D), f)
    for l in range(4):
        norms[l * 4 + 0] = inp["norm_mix_pre"][l]
        norms[l * 4 + 1] = inp["norm_mix_post"][l]
        norms[l * 4 + 2] = inp["norm_ffn_pre"][l]
        norms[l * 4 + 3] = inp["norm_ffn_post"][l]
    norms[16] = inp["kv_norm"]
    sh["norms"] = np.ascontiguousarray(norms.reshape(17, KC, 128).transpose(2, 0, 1))
    for l in range(2):
        sh["w_in%d" % l] = _kmajor(inp["mlstm_w_in"][l])
        sh["w_mout%d" % l] = _kmajor(inp["mlstm_w_out"][l])
        sh["w_qg%d" % l] = _kmajor(inp["fox_w_qg"][l])
        sh["w_fout%d" % l] = np.ascontiguousarray(inp["fox_w_out"][l].reshape(16, 64, D).transpose(1, 0, 2))
    sh["bgate"] = np.ascontiguousarray(np.broadcast_to(inp["mlstm_b_gate"][None], (128, 2, 16))).astype(f)
    sh["hnorm"] = np.ascontiguousarray(np.broadcast_to(inp["mlstm_norm"][None], (128, 2, D))).astype(f)
    sh["kvw"] = _kmajor(inp["kv_w"])
    sh["kvb"] = np.ascontiguousarray(np.broadcast_to(inp["kv_b_f"][None], (128, 16))).astype(f)
    for l in range(4):
        sh["w_up%d" % l] = _kmajor(inp["ffn_w_up"][l])
        sh["w_dn%d" % l] = _kmajor(inp["ffn_w_down"][l])
    cw = np.zeros((4, 4, 2 * DFF), f)
    cw[:, 0:3] = inp["ffn_conv_w"]
    cw[:, 3] = inp["ffn_conv_b"]
    sh["convw"] = np.ascontiguousarray(cw.reshape(4, 4, MFF, 128).transpose(3, 0, 2, 1))
    sh["ident"] = np.eye(128, dtype=f)
    s = np.arange(128)
    sh["tri"] = (s[:, None] <= s[None, :]).astype(f)
    sh["negm"] = np.where(s[:, None] <= s[None, :], 0.0, NEG).astype(f)
    return sh


def x_to_core(xs):
    n, S, _ = xs.shape
    return np.ascontiguousarray(xs.reshape(n * S, KC, 128).transpose(2, 1, 0))


def core_to_x(o, nseq, S):
    return np.ascontiguousarray(o.transpose(2, 1, 0).reshape(nseq, S, D))


def kernel(**inputs):
    inp = {k: np.asarray(v, dtype=np.float32) for k, v in inputs.items()}
    x = inp["x"]
    B, S, _ = x.shape
    ncores = 8
    nseq = B // ncores
    nc = build(nseq, S)
    sh = prep_shared(inp)
    in_maps = []
    for c in range(ncores):
        m = dict(sh)
        m["xT"] = x_to_core(x[c * nseq:(c + 1) * nseq])
        in_maps.append(m)
    res = run_bass_kernel_spmd(nc, in_maps, core_ids=list(range(ncores)))
    out = np.empty_like(x)
    for c in range(ncores):
        out[c * nseq:(c + 1) * nseq] = core_to_x(res.results[c]["outT"], nseq, S)
    return out
```

```python
from contextlib import ExitStack
import numpy as np
import ml_dtypes
import concourse.bass as bass
import concourse.mybir as mybir
from concourse.bass_utils import run_bass_kernel_spmd

F32 = mybir.dt.float32
BF16 = mybir.dt.bfloat16
AF = mybir.ActivationFunctionType
ALU = mybir.AluOpType
AX = mybir.AxisListType

D = 1024
KC = 8
T = 512
NCH = T // 128
DFF = 2816
MFF = 44
EPS = 1e-6
NLAYER = 4
MIN = 3088
NEG = -30000.0
DEBUG = False


class Buf:
    __slots__ = ("name", "w", "r", "excl")

    def __init__(self, name, excl=False):
        self.name = name
        self.w = None
        self.r = {}
        self.excl = excl


class SemPool:
    def __init__(self, sems):
        self.sems = sems
        self.tot = [0] * len(sems)
        self.i = 0

    def next(self):
        i = self.i
        self.i = (i + 1) % len(self.sems)
        return i


class Prog:
    def __init__(self, nc, es):
        self.nc = nc
        self.eng = {"pe": nc.tensor, "act": nc.scalar, "dve": nc.vector, "pool": nc.gpsimd, "sp": nc.sync}
        self.sem = {}
        self.cnt = {}
        self.seen = {}
        self.semobj = {}
        for e in self.eng:
            s = es.enter_context(nc.semaphore("sem_" + e))
            self.semobj["E" + e] = s
            self.cnt[e] = 0
            self.seen[e] = {}
        self.es = es
        self.npool = 0
        self.nwait = 0

    def pool(self, n):
        sems = []
        for i in range(n):
            key = "P%d_%d" % (self.npool, i)
            s = self.es.enter_context(self.nc.semaphore(key))
            self.semobj[key] = s
            sems.append(key)
        self.npool += 1
        return SemPool(sems)

    def wait(self, e, ev):
        if ev is None:
            return
        key, val = ev
        if val <= 0:
            return
        if e == "pe" and key == "Epe":
            return
        if self.seen[e].get(key, 0) >= val:
            return
        self.eng[e].wait_ge(self.semobj[key], val)
        self.seen[e][key] = val
        self.nwait += 1

    def deps(self, e, reads, writes):
        for b in reads:
            self.wait(e, b.w)
            if b.excl:
                for k, v in b.r.items():
                    self.wait(e, (k, v))
        for b in writes:
            self.wait(e, b.w)
            for k, v in b.r.items():
                self.wait(e, (k, v))

    def mark(self, ev, reads, writes):
        for b in reads:
            b.r[ev[0]] = ev[1]
        for b in writes:
            b.w = ev
            b.r = {}

    def op(self, e, fn, reads=(), writes=()):
        self.deps(e, reads, writes)
        ins = fn(self.eng[e])
        self.cnt[e] += 1
        ins.then_inc(self.semobj["E" + e], 1)
        self.mark(("E" + e, self.cnt[e]), reads, writes)

    def mm(self, items, writes):
        ev = ("Epe", self.cnt["pe"] + 1)
        ins = None
        allr = []
        for (out, lhsT, rhs, st, sp, reads) in items:
            self.deps("pe", reads, writes)
            ins = self.nc.tensor.matmul(out, lhsT, rhs, start=st, stop=sp)
            for b in reads:
                if b not in allr:
                    allr.append(b)
        self.cnt["pe"] += 1
        ins.then_inc(self.semobj["Epe"], 1)
        self.mark(ev, allr, writes)

    def transposes(self, items, writes):
        ev = ("Epe", self.cnt["pe"] + 1)
        ins = None
        allr = []
        for (out, in_, ident, reads) in items:
            self.deps("pe", reads, writes)
            ins = self.nc.tensor.transpose(out, in_, ident)
            for b in reads:
                if b not in allr:
                    allr.append(b)
        self.cnt["pe"] += 1
        ins.then_inc(self.semobj["Epe"], 1)
        self.mark(ev, allr, writes)

    def dma(self, q, out, in_, reads, writes, pool):
        i = pool.next()
        key = pool.sems[i]
        self.wait(q, (key, pool.tot[i]))
        self.deps(q, reads, writes)
        ins = self.eng[q].dma_start(out=out, in_=in_)
        ins.then_inc(self.semobj[key], 16)
        pool.tot[i] += 16
        ev = (key, pool.tot[i])
        self.mark(ev, reads, writes)
        return ev


class Rot:
    def __init__(self, items):
        self.items = items
        self.i = 0

    def next(self):
        it = self.items[self.i]
        self.i = (self.i + 1) % len(self.items)
        return it


def build(nseq, seqlen, layers=(0, 1, 2, 3), do_kv=True, parts="mf"):
    assert seqlen % T == 0
    ntile = seqlen // T
    nblk = seqlen // 128
    ntok = nseq * seqlen
    nc = bass.Bass("TRN2", target_bir_lowering=False)
    es = ExitStack()
    P = Prog(nc, es)

    def dram_in(name, shape, dt=F32):
        return nc.dram_tensor(name, list(shape), dt, kind="ExternalInput").ap()

    xT_d = dram_in("xT", [128, KC, ntok])
    outT_d = nc.dram_tensor("outT", [128, KC, ntok], F32, kind="ExternalOutput").ap()
    norms_d = dram_in("norms", [128, 17, KC])
    w_in_d = [dram_in("w_in%d" % l, [128, KC, MIN]) for l in range(2)]
    w_mout_d = [dram_in("w_mout%d" % l, [128, KC, D]) for l in range(2)]
    bgate_d = dram_in("bgate", [128, 2, 16])
    hnorm_d = dram_in("hnorm", [128, 2, D])
    kvw_d = dram_in("kvw", [128, KC, 2064])
    kvb_d = dram_in("kvb", [128, 16])
    w_qg_d = [dram_in("w_qg%d" % l, [128, KC, 2 * D]) for l in range(2)]
    w_fout_d = [dram_in("w_fout%d" % l, [64, 16, D]) for l in range(2)]
    w_up_d = [dram_in("w_up%d" % l, [128, KC, 2 * DFF]) for l in range(4)]
    w_dn_d = [dram_in("w_dn%d" % l, [128, 22, D]) for l in range(4)]
    convw_d = dram_in("convw", [128, 4, MFF, 4])
    ident_d = dram_in("ident", [128, 128])
    tri_d = dram_in("tri", [128, 128])
    negm_d = dram_in("negm", [128, 128])
    kscr_d = nc.dram_tensor("kscr", [nseq * nblk, 65, 16, 128], BF16, kind="Internal").ap()
    vscr_d = nc.dram_tensor("vscr", [nseq * nblk, 128, 16, 65], BF16, kind="Internal").ap()

    def sb(name, shape, dt):
        return es.enter_context(nc.sbuf_tensor(name, list(shape), dt))

    xT = sb("xT_s", [128, KC, T], F32); xT_bs = [Buf("xT%d" % i) for i in range(KC)]
    hT = sb("hT_s", [128, KC, T], BF16); hT_bs = [Buf("hT%d" % i) for i in range(KC)]
    yT = sb("yT_s", [128, KC, T], F32); yT_b = Buf("yT")
    sq = sb("sq_s", [128, KC, T], BF16); sq_bs = [Buf("sq%d" % i) for i in range(KC)]
    rstd = sb("rstd_s", [128, T], F32); rstd_b = Buf("rstd")
    lnt = rstd; lnt_b = rstd_b
    norms = sb("norms_s", [128, 17, KC], F32); norms_b = Buf("norms")
    gT = sb("gT_s", [128, 22, T], BF16); gT_bs = [Buf("gT%d" % i) for i in range(22)]
    convw = sb("convw_s", [128, 4, MFF, 4], F32); convw_b = Buf("convw")
    tails = sb("tails_s", [128, 4, MFF, 2], F32); tails_b = [[Buf("tail%d_%d" % (l, m)) for m in range(MFF)] for l in range(4)]
    NUE = 4
    ue = sb("ue_s", [128, NUE, T + 2], F32); ue_r = Rot([(ue[:, i, :], Buf("ue%d" % i)) for i in range(NUE)])
    yc = sb("yc_s", [128, NUE, T], F32); yc_r = Rot([(yc[:, i, :], Buf("yc%d" % i)) for i in range(NUE)])
    tmpn = yc; tmpn_bs = [yc_r.items[0][1], yc_r.items[1][1]]
    NWB = 3
    wb = sb("wb_s", [128, NWB, 4096], BF16); wb_r = Rot([(wb[:, i, :], Buf("wb%d" % i)) for i in range(NWB)])
    wsm = sb("wsm_s", [128, 2, KC, 16], BF16); wsm_r = Rot([(wsm[:, i], Buf("wsm%d" % i)) for i in range(2)])
    ident = sb("ident_s", [128, 128], BF16); ident_b = Buf("ident")
    trif = sb("trif_s", [128, 128], F32); onesf = sb("onesf_s", [128, 128], F32)
    onesb = sb("onesb_s", [128, 128], BF16)
    mask4 = sb("mask4_s", [128, 4, 128], BF16)
    negm = sb("negm_s", [128, 128], BF16)
    const_b = Buf("consts")
    qTf = sb("qT_s", [65, 8, T], BF16); qT = qTf[0:64]; qT_b = Buf("qT")
    kTmf = sb("kTm_s", [65, 8, T], BF16); kTm = kTmf[0:64]; kTm_b = Buf("kTm")
    ktok = sb("ktok_s", [128, NCH, 512], BF16); ktok_b = [Buf("ktok%d" % c) for c in range(NCH)]
    vtok = sb("vtok_s", [128, NCH, D], BF16); vtok_b = [Buf("vtok%d" % c) for c in range(NCH)]
    sotok = sb("sotok_s", [128, NCH, D], BF16); sotok_b = [Buf("sotok%d" % c) for c in range(NCH)]
    bgate = sb("bgate_s", [128, 2, 16], F32)
    hnorm = sb("hnorm_s", [128, 2, D], F32)
    Cst = sb("Cst_s", [64, 2, 8, 129], F32); Cst_b = [[Buf("C%d_%d" % (l, h)) for h in range(8)] for l in range(2)]
    Cw = sb("Cw_s", [64, 8, 129], BF16); Cw_b = [Buf("Cw%d" % h) for h in range(8)]
    Est = sb("Est_s", [128, 2, 8], F32); Est_b = [Buf("E%d" % l) for l in range(2)]
    gsm = sb("gsm_s", [128, 16, 16], F32); gsm_b = Buf("gsm"); gW_b = Buf("gW"); gOut_b = Buf("gOut")
    vext = sb("vext_s", [128, 1, 8, 129], BF16); vext_r = Rot([(vext[:, i], [Buf("vext%d_%d" % (i, h)) for h in range(8)]) for i in range(1)])
    stm = sb("stm_s", [128, 2, 4, 128], BF16); stm_r = Rot([(stm[:, i], Buf("stm%d" % i)) for i in range(2)])
    gw = sb("gw_s", [128, D], F32); gw_b = Buf("gw")
    hstok = sb("hstok_s", [128, D], BF16); hstok_b = Buf("hstok")
    junk = sb("junk_s", [128, 128], F32); junk_b = Buf("junk")
    y16 = yT[:].bitcast(BF16).rearrange("p k (a t) -> p (k a) t", t=T)
    print("y16", y16.shape)
    kTs = y16[0:65]; kTs_b = yT_b
    vts = sb("vts_s", [128, NCH, 16, 65], BF16); vts_b = Buf("vts")
    kvb = sb("kvb_s", [128, 16], F32)
    negc = sb("negc_s", [128, nblk, 16], F32); negc_b = Buf("negc")
    ncprev = sb("ncprev_s", [128, 16], F32); ncprev_b = Buf("ncprev")
    Caug = sb("Caug_s", [128, 16, 65], BF16); Caug_b = Buf("Caug")
    QTa = y16[0:65]; QTa_b = yT_b; QTrow_b = yT_b
    oT = gT[0:64, 0:16, :]; oT_bs = gT_bs[0:16]
    NST = 1
    NPT = 2
    pT = sb("pT_s", [128, NPT, T], BF16); pT_r = Rot([(pT[:, i, :], Buf("pT%d" % i)) for i in range(NPT)])
    rden = sb("rden_s", [65, 1, T], F32); rden_b = Buf("rden")
    rhl = sb("rhl_s", [65, 2, T], BF16); rhl_b = Buf("rhl")
    numsb = sb("numsb_s", [64, T], F32); numsb_b = Buf("numsb")

    NPS = 7
    ps_r = Rot([])
    for i in range(NPS):
        pt = es.enter_context(nc.psum_tensor("ps%d" % i, [128, 512], F32))
        ps_r.items.append((pt, Buf("ps%d" % i, excl=True)))
    pst = es.enter_context(nc.psum_tensor("pst", [128, 1024], BF16)); pst_b = Buf("pst", excl=True)

    wpool = P.pool(8)
    cpool = P.pool(4)
    xpool = P.pool(2)
    opool = P.pool(2)
    kvwpool = P.pool(8)
    stpool = P.pool(8)

    cev = []
    cev.append(P.dma("sp", norms[:], norms_d, [], [norms_b], cpool))
    cev.append(P.dma("sp", convw[:], convw_d, [], [convw_b], cpool))
    cev.append(P.dma("sp", trif[:], tri_d, [], [const_b], cpool))
    cev.append(P.dma("sp", bgate[:], bgate_d, [], [const_b], cpool))
    cev.append(P.dma("sp", hnorm[:], hnorm_d, [], [const_b], cpool))
    cev.append(P.dma("sp", kvb[:], kvb_d, [], [const_b], cpool))
    cev.append(P.dma("pool", negm[:], negm_d, [], [const_b], cpool))
    cev.append(P.dma("pool", ident[:], ident_d, [], [ident_b], cpool))
    for e in ("pe", "act", "dve"):
        for ev in cev:
            P.wait(e, ev)
    P.op("dve", lambda v: v.memset(onesf[:], 1.0), [], [const_b])
    P.op("dve", lambda v: v.memset(onesb[:], 1.0), [], [const_b])
    for j in range(4):
        P.op("dve", lambda v, j=j: v.tensor_copy(out=mask4[:, j, :], in_=trif[:]), [], [const_b])
    P.op("dve", lambda v: v.memset(Caug[:], 0.0), [], [Caug_b])
    P.op("dve", lambda v: v.memset(vts[:], 1.0), [], [vts_b])

    prepool = P.pool(16)
    wsrc = {}

    def convert(name, src, shape):
        dst = nc.dram_tensor(name + "_bf", list(shape), BF16, kind="Internal").ap()
        rows = shape[0] * shape[1]
        s2 = src.rearrange("p k n -> (p k) n")
        d2 = dst.rearrange("p k n -> (p k) n")
        bufs = []
        R = 32
        for r0 in range(0, rows, R):
            b = Buf(name + "_c%d" % r0)
            r1 = min(rows, r0 + R)
            P.dma("pool", d2[r0:r1, :], s2[r0:r1, :], [], [b], prepool)
            bufs.append(b)
        wsrc[name] = (dst, bufs)

    for l in layers:
        if "m" in parts and l < 2:
            convert("w_in%d" % l, w_in_d[l], [128, KC, MIN])
            convert("w_mout%d" % l, w_mout_d[l], [128, KC, D])
        if "m" in parts and l >= 2:
            if "kvw" not in wsrc and do_kv:
                convert("kvw", kvw_d, [128, KC, 2064])
            convert("w_qg%d" % (l - 2), w_qg_d[l - 2], [128, KC, 2 * D])
            convert("w_fout%d" % (l - 2), w_fout_d[l - 2], [64, 16, D])
        if "f" in parts:
            convert("w_up%d" % l, w_up_d[l], [128, KC, 2 * DFF])
            convert("w_dn%d" % l, w_dn_d[l], [128, 22, D])
        if l == 1 and do_kv and "kvw" not in wsrc:
            convert("kvw", kvw_d, [128, KC, 2064])

    def WS(name):
        return wsrc[name]

    wstate = {"pending": [], "specs": [], "pos": 0, "issued": 0}

    def wspecs_for_tile():
        specs = []

        def add(tag, name, c0, n, npart, k):
            dst, bufs = WS(name)
            specs.append((tag, dst[:, :, c0:c0 + n], npart, k, n, bufs))

        def add_kv():
            for c0 in range(0, 2048, 512):
                add("kv_%d" % c0, "kvw", c0, 512, 128, KC)
            add("kvf", "kvw", 2048, 16, 128, KC)

        if do_kv and 1 not in layers:
            add_kv()
        for l in layers:
            if "m" not in parts:
                pass
            elif l < 2:
                for c0 in range(0, 3072, 512):
                    add("min%d_%d" % (l, c0), "w_in%d" % l, c0, 512, 128, KC)
                add("mif%d" % l, "w_in%d" % l, 3072, 16, 128, KC)
                for c0 in (0, 512):
                    add("mout%d_%d" % (l, c0), "w_mout%d" % l, c0, 512, 128, KC)
            else:
                j = l - 2
                for c0 in range(0, 2048, 512):
                    add("qg%d_%d" % (j, c0), "w_qg%d" % j, c0, 512, 128, KC)
                for c0 in range(0, 1024, 256):
                    add("fout%d_%d" % (j, c0), "w_fout%d" % j, c0, 256, 64, 16)
            if "f" in parts:
                for c0 in range(0, 2 * DFF, 512):
                    add("up%d_%d" % (l, c0), "w_up%d" % l, c0, 512, 128, KC)
                for c0 in range(0, D, 128):
                    add("dn%d_%d" % (l, c0), "w_dn%d" % l, c0, 128, 128, 22)
            if l == 1 and do_kv:
                add_kv()
        return specs

    tile_specs = wspecs_for_tile()
    all_specs = tile_specs * (nseq * ntile)
    LA = 2

    def w_issue():
        i = wstate["issued"]
        if i >= len(all_specs):
            return
        tag, src, npart, k, n, rbufs = all_specs[i]
        if n == 16:
            ap, b = wsm_r.next()
            dst = ap[:, :, :]
        else:
            ap, b = wb_r.next()
            dst = ap[0:npart, 0:k * n].rearrange("p (k n) -> p k n", n=n)
        P.dma("pool", dst, src, rbufs, [b], wpool)
        wstate["pending"].append((tag, dst, b))
        wstate["issued"] = i + 1

    def w_get(tag):
        while wstate["issued"] < min(len(all_specs), wstate["pos"] + LA + 1):
            w_issue()
        t, dst, b = wstate["pending"].pop(0)
        assert t == tag, (t, tag)
        wstate["pos"] += 1
        return dst, b

    def evac(i, fn_act, fn_dve, reads, writes):
        if i % 2 == 0:
            P.op("act", fn_act, reads, writes)
        else:
            P.op("dve", fn_dve, reads, writes)

    def rmsnorm_stats(src, src_bs, nidx):
        pt, pb = ps_r.next()
        for kc in range(KC):
            sb_ = src_bs[kc] if isinstance(src_bs, list) else src_bs
            P.op("act", lambda a, kc=kc: a.activation(out=sq[:, kc, :], in_=src[:, kc, :], func=AF.Square), [sb_], [sq_bs[kc]])
        P.mm([(pt[:, :], onesb[:, :], sq[:, kc, :], kc == 0, kc == KC - 1, [sq_bs[kc]]) for kc in range(KC)], [pb])
        P.op("act", lambda a: a.activation(out=lnt[:], in_=pt[:, :], func=AF.Ln, scale=1.0 / D, bias=EPS), [pb], [lnt_b])
        P.op("act", lambda a: a.activation(out=rstd[:], in_=lnt[:], func=AF.Exp, scale=-0.5), [lnt_b], [rstd_b])

    def prenorm(nidx):
        rmsnorm_stats(xT, xT_bs, nidx)
        for kc in range(KC):
            P.op("dve", lambda v, kc=kc: v.scalar_tensor_tensor(out=hT[:, kc, :], in0=xT[:, kc, :], scalar=norms[:, nidx, kc:kc + 1],
                                                             in1=rstd[:], op0=ALU.mult, op1=ALU.mult),
                 [xT_bs[kc], rstd_b, norms_b], [hT_bs[kc]])

    def postnorm_residual(nidx):
        rmsnorm_stats(yT, yT_b, nidx)
        for kc in range(KC):
            tb = tmpn_bs[kc % 2]
            P.op("dve", lambda v, kc=kc: v.tensor_tensor(out=tmpn[:, kc % 2, :], in0=yT[:, kc, :], in1=rstd[:], op=ALU.mult),
                 [yT_b, rstd_b], [tb])
            P.op("dve", lambda v, kc=kc: v.scalar_tensor_tensor(out=xT[:, kc, :], in0=tmpn[:, kc % 2, :], scalar=norms[:, nidx, kc:kc + 1],
                                                                 in1=xT[:, kc, :], op0=ALU.mult, op1=ALU.add),
                 [tb, norms_b], [xT_bs[kc]])

    def proj_fm(wdst, w_b, m_lo, m_n, src, src_b, kcn, out_fn, npart=128, mwidth=128):
        for mi in range(m_n):
            pt, pb = ps_r.next()
            c0 = m_lo + mi * mwidth
            P.mm([(pt[0:mwidth, :], wdst[0:npart, kc, c0:c0 + mwidth], src[0:npart, kc, :], kc == 0, kc == kcn - 1,
                   [w_b, (src_b[kc] if isinstance(src_b, list) else src_b)])
                  for kc in range(kcn)], [pb])
            out_fn(mi, pt, pb)

    def ffn(l):
        pending = [None]
        for blk in range(11):
            wdst, w_b = w_get("up%d_%d" % (l, blk * 512))
            for mi in range(4):
                m = blk * 4 + mi
                pt, pb = ps_r.next()
                P.mm([(pt[:, :], wdst[:, kc, mi * 128:(mi + 1) * 128], hT[:, kc, :], kc == 0, kc == KC - 1, [w_b, hT_bs[kc]])
                      for kc in range(KC)], [pb])
                uea, ueb = ue_r.next()
                yca, ycb = yc_r.next()
                tb = tails_b[l][m]
                P.op("dve", lambda v: v.tensor_copy(out=uea[:, 0:2], in_=tails[:, l, m, :]), [tb], [ueb])
                P.op("act", lambda a: a.copy(out=uea[:, 2:T + 2], in_=pt[:, :]), [pb], [ueb])
                P.op("act", lambda a: a.activation(out=yca, in_=uea[:, 2:T + 2], func=AF.Identity, scale=convw[:, l, m, 2:3],
                                                   bias=convw[:, l, m, 3:4]), [ueb, convw_b], [ycb])

                def stage_b(m=m, uea=uea, ueb=ueb, yca=yca, ycb=ycb):
                    P.op("dve", lambda v: v.scalar_tensor_tensor(out=yca, in0=uea[:, 1:T + 1], scalar=convw[:, l, m, 1:2], in1=yca,
                                                                 op0=ALU.mult, op1=ALU.add), [ueb, convw_b, ycb], [ycb])
                    P.op("dve", lambda v: v.scalar_tensor_tensor(out=yca, in0=uea[:, 0:T], scalar=convw[:, l, m, 0:1], in1=yca,
                                                                 op0=ALU.mult, op1=ALU.add), [ueb, convw_b, ycb], [ycb])
                    if m < 22:
                        P.op("act", lambda a: a.activation(out=gT[:, m, :], in_=yca, func=AF.Gelu_apprx_tanh), [ycb], [gT_bs[m]])
                    else:
                        mm_ = m - 22
                        P.op("dve", lambda v: v.tensor_tensor(out=gT[:, mm_, :], in0=gT[:, mm_, :], in1=yca, op=ALU.mult),
                             [ycb], [gT_bs[mm_]])

                if pending[0] is not None:
                    pending[0]()
                pending[0] = stage_b
                P.op("dve", lambda v: v.tensor_copy(out=tails[:, l, m, :], in_=uea[:, T:T + 2]), [ueb], [tb])
        pending[0]()
        for mo in range(8):
            wdst, w_b = w_get("dn%d_%d" % (l, mo * 128))
            pt, pb = ps_r.next()
            P.mm([(pt[:, :], wdst[:, kc, :], gT[:, kc, :], kc == 0, kc == 21, [w_b, gT_bs[kc]]) for kc in range(22)], [pb])
            evac(mo, lambda a: a.copy(out=yT[:, mo, :], in_=pt[:, :]), lambda v: v.tensor_copy(out=yT[:, mo, :], in_=pt[:, :]), [pb], [yT_b])

    def mlstm(l, first_tile):
        if first_tile:
            P.op("dve", lambda v: v.memset(Cst[:, l], 0.0), [], Cst_b[l])
            P.op("dve", lambda v: v.memset(Est[:, l, :], 1.0), [], [Est_b[l]])
        wdst, w_b = w_get("min%d_%d" % (l, 0))
        proj_fm(wdst, w_b, 0, 8, hT, hT_bs, KC,
                lambda h, pt, pb: evac(h, lambda a: a.mul(out=qT[:, h, :], in_=pt[0:64, :], mul=0.125),
                                       lambda v: v.tensor_scalar(out=qT[:, h, :], in0=pt[0:64, :], scalar1=0.125, scalar2=None, op0=ALU.mult),
                                       [pb], [qT_b]), mwidth=64)
        wdst, w_b = w_get("min%d_%d" % (l, 512))
        proj_fm(wdst, w_b, 0, 8, hT, hT_bs, KC,
                lambda h, pt, pb: evac(h + 1, lambda a: a.copy(out=kTm[:, h, :], in_=pt[0:64, :]),
                                       lambda v: v.tensor_copy(out=kTm[:, h, :], in_=pt[0:64, :]), [pb], [kTm_b]), mwidth=64)
        for c in range(NCH):
            pt, pb = ps_r.next()
            P.mm([(pt[:, :], hT[:, kc, c * 128:(c + 1) * 128], wdst[:, kc, :], kc == 0, kc == KC - 1, [w_b, hT_bs[kc]]) for kc in range(KC)], [pb])
            evac(c, lambda a: a.copy(out=ktok[:, c, :], in_=pt[:, :]), lambda v: v.tensor_copy(out=ktok[:, c, :], in_=pt[:, :]), [pb], [ktok_b[c]])
        for half in range(2):
            wdst, w_b = w_get("min%d_%d" % (l, 1024 + half * 512))
            for c in range(NCH):
                pt, pb = ps_r.next()
                P.mm([(pt[:, :], hT[:, kc, c * 128:(c + 1) * 128], wdst[:, kc, :], kc == 0, kc == KC - 1, [w_b, hT_bs[kc]]) for kc in range(KC)], [pb])
                evac(c + half, lambda a: a.copy(out=vtok[:, c, half * 512:(half + 1) * 512], in_=pt[:, :]),
                     lambda v: v.tensor_copy(out=vtok[:, c, half * 512:(half + 1) * 512], in_=pt[:, :]), [pb], [vtok_b[c]])
        for half in range(2):
            wdst, w_b = w_get("min%d_%d" % (l, 2048 + half * 512))
            for c in range(NCH):
                pt, pb = ps_r.next()
                P.mm([(pt[:, :], hT[:, kc, c * 128:(c + 1) * 128], wdst[:, kc, :], kc == 0, kc == KC - 1, [w_b, hT_bs[kc]]) for kc in range(KC)], [pb])
                P.op("act", lambda a: a.activation(out=sotok[:, c, half * 512:(half + 1) * 512], in_=pt[:, :], func=AF.Sigmoid), [pb], [sotok_b[c]])
        wif, wif_b = w_get("mif%d" % l)
        G = gsm
        for c in range(NCH):
            cs = slice(c * 128, (c + 1) * 128)
            pg, pgb = ps_r.next()
            P.mm([(pg[:, 0:16], hT[:, kc, cs], wif[:, kc, :], kc == 0, kc == KC - 1, [wif_b, hT_bs[kc]]) for kc in range(KC)], [pgb])
            P.op("dve", lambda v: v.tensor_tensor(out=G[:, 0, :], in0=pg[:, 0:16], in1=bgate[:, l, :], op=ALU.add), [pgb], [gsm_b])
            P.op("act", lambda a: a.activation(out=G[:, 1, 0:8], in_=G[:, 0, 8:16], func=AF.Exp, scale=-1.0), [gsm_b], [gsm_b])
            P.op("act", lambda a: a.activation(out=G[:, 1, 0:8], in_=G[:, 1, 0:8], func=AF.Ln, bias=1.0), [gsm_b], [gsm_b])
            pc, pcb = ps_r.next()
            P.mm([(pc[:, 0:8], trif[:, :], G[:, 1, 0:8], True, True, [gsm_b]),
                  (pc[:, 8:16], onesf[:, :], G[:, 1, 0:8], True, True, [gsm_b])], [pcb])
            P.op("dve", lambda v: v.tensor_tensor(out=G[:, 2, 0:8], in0=pc[:, 0:8], in1=G[:, 0, 0:8], op=ALU.add), [pcb, gsm_b], [gsm_b])
            P.op("act", lambda a: a.activation(out=G[:, 1, 8:16], in_=G[:, 2, 0:8], func=AF.Exp), [gsm_b], [gsm_b])
            P.op("act", lambda a: a.activation(out=G[:, 5, 0:8], in_=pc[:, 8:16], func=AF.Exp, scale=-1.0), [pcb], [gsm_b])
            P.op("act", lambda a: a.activation(out=G[:, 5, 8:16], in_=pc[:, 0:8], func=AF.Exp), [pcb], [gsm_b])
            pe_, peb = ps_r.next()
            P.mm([(pe_[:, 0:8], onesf[:, :], G[:, 1, 8:16], True, True, [gsm_b])], [peb])
            P.op("dve", lambda v: v.tensor_tensor(out=G[:, 3, 0:8], in0=pe_[:, 0:8], in1=Est[:, l, :], op=ALU.add), [peb, Est_b[l]], [gsm_b])
            P.op("dve", lambda v: v.reciprocal(out=G[:, 3, 8:16], in_=G[:, 3, 0:8]), [gsm_b], [gsm_b])
            P.op("dve", lambda v: v.tensor_tensor(out=G[:, 4, 0:8], in0=G[:, 1, 8:16], in1=G[:, 3, 8:16], op=ALU.mult), [gsm_b], [gW_b])
            P.op("dve", lambda v: v.tensor_tensor(out=G[:, 4, 8:16], in0=Est[:, l, :], in1=G[:, 3, 8:16], op=ALU.mult), [gsm_b, Est_b[l], gW_b], [gW_b])
            P.op("dve", lambda v: v.tensor_tensor(out=Est[:, l, :], in0=G[:, 5, 0:8], in1=G[:, 3, 0:8], op=ALU.mult), [gsm_b], [Est_b[l]])
            P.op("dve", lambda v: v.tensor_tensor(out=G[:, 6, 0:8], in0=G[:, 5, 8:16], in1=G[:, 3, 8:16], op=ALU.mult), [gsm_b], [gsm_b])
            for h in range(8):
                P.op("act", lambda a, h=h: a.activation(out=Cw[:, h, :], in_=Cst[:, l, h, :], func=AF.Identity, scale=G[0:64, 4, 8 + h:9 + h]),
                     [Cst_b[l][h], gW_b], [Cw_b[h]])
            vxa, vxb = vext_r.next()
            for h in range(8):
                P.op("act", lambda a, h=h: a.activation(out=vxa[:, h, 0:128], in_=vtok[:, c, h * 128:(h + 1) * 128], func=AF.Identity,
                                                        scale=G[:, 4, h:h + 1]), [vtok_b[c], gW_b], [vxb[h]])
            P.op("dve", lambda v: v.tensor_copy(out=vxa[:, :, 128], in_=G[:, 4, 0:8]), [gW_b], vxb)
            sts = []
            for hg in range(2):
                pS, pSb = ps_r.next()
                P.mm([(pS[:, j * 128:(j + 1) * 128], kTm[:, hg * 4 + j, cs], qT[:, hg * 4 + j, cs], True, True, [kTm_b, qT_b]) for j in range(4)], [pSb])
                sta, stb = stm_r.next()
                P.op("dve", lambda v, sta=sta, pS=pS: v.tensor_tensor(out=sta[:, :, :], in0=pS[:, :].rearrange("p (j t) -> p j t", t=128), in1=mask4[:, :, :], op=ALU.mult),
                     [pSb, const_b], [stb])
                sts.append((sta, stb))
            nums = {}
            for grp in ((0, 1, 2), (3, 4, 5), (6, 7)):
                pn, pnb = ps_r.next()
                items = []
                for j, h in enumerate(grp):
                    sta, stb = sts[h // 4]
                    o_ = pn[:, j * 129:(j + 1) * 129]
                    items.append((o_, sta[:, h % 4, :], vxa[:, h, :], True, False, [stb, vxb[h]]))
                    items.append((o_, qT[:, h, cs], Cw[:, h, :], False, True, [qT_b, Cw_b[h]]))
                    nums[h] = (pn, pnb, j)
                P.mm(items, [pnb])
            for hg in range(3):
                grp = ((0, 1, 2), (3, 4, 5), (6, 7))[hg]
                pd, pdb = ps_r.next()
                P.mm([(pd[0:64, j * 129:(j + 1) * 129], ktok[:, c, h * 64:(h + 1) * 64], vxa[:, h, :], True, True, [ktok_b[c], vxb[h]]) for j, h in enumerate(grp)], [pdb])
                for j, h in enumerate(grp):
                    P.op("dve", lambda v, j=j, h=h, pd=pd: v.scalar_tensor_tensor(out=Cst[:, l, h, :], in0=Cst[:, l, h, :], scalar=G[0:64, 4, 8 + h:9 + h],
                                                                                   in1=pd[0:64, j * 129:(j + 1) * 129], op0=ALU.mult, op1=ALU.add),
                         [pdb, gW_b, Cw_b[h]], [Cst_b[l][h]])
            for h in range(8):
                pn, pnb, j = nums[h]
                P.op("act", lambda a, h=h, pn=pn, j=j: a.activation(out=G[:, 6, 8 + h:9 + h], in_=pn[:, j * 129 + 128:j * 129 + 129], func=AF.Abs),
                     [pnb], [gOut_b])
            P.op("dve", lambda v: v.tensor_tensor(out=G[:, 7, 0:8], in0=G[:, 6, 8:16], in1=G[:, 6, 0:8], op=ALU.max), [gsm_b, gOut_b], [gOut_b])
            P.op("dve", lambda v: v.reciprocal(out=G[:, 7, 8:16], in_=G[:, 7, 0:8]), [gOut_b], [gOut_b])
            P.op("dve", lambda v: v.memset(G[:, 8, :], 0.0), [], [gOut_b])
            for h in range(8):
                pn, pnb, j = nums[h]
                P.op("act", lambda a, h=h, pn=pn, j=j: a.activation(out=junk[:, :], in_=pn[:, j * 129:j * 129 + 128], func=AF.Square, scale=G[:, 7, 8 + h:9 + h],
                                                                    accum_out=G[:, 8, h:h + 1]), [pnb, gOut_b], [junk_b, gOut_b])
            P.op("act", lambda a: a.activation(out=G[:, 9, 0:8], in_=G[:, 8, 0:8], func=AF.Ln, scale=1.0 / 128, bias=EPS), [gOut_b], [gOut_b])
            P.op("act", lambda a: a.activation(out=G[:, 8, 8:16], in_=G[:, 9, 0:8], func=AF.Exp, scale=-0.5), [gOut_b], [gOut_b])
            P.op("dve", lambda v: v.tensor_tensor(out=G[:, 9, 8:16], in0=G[:, 8, 8:16], in1=G[:, 7, 8:16], op=ALU.mult), [gOut_b], [gOut_b])
            P.op("dve", lambda v: v.tensor_tensor(out=gw[:, :], in0=sotok[:, c, :], in1=hnorm[:, l, :], op=ALU.mult), [sotok_b[c], const_b], [gw_b])
            for h in range(8):
                pn, pnb, j = nums[h]
                P.op("dve", lambda v, h=h, pn=pn, j=j: v.scalar_tensor_tensor(out=hstok[:, h * 128:(h + 1) * 128], in0=pn[:, j * 129:j * 129 + 128],
                                                                               scalar=G[:, 9, 8 + h:9 + h], in1=gw[:, h * 128:(h + 1) * 128], op0=ALU.mult, op1=ALU.mult),
                     [pnb, gOut_b, gw_b], [hstok_b])
            P.transposes([(pst[:, h * 128:(h + 1) * 128], hstok[:, h * 128:(h + 1) * 128], ident[:, :], [hstok_b, ident_b]) for h in range(8)], [pst_b])
            P.op("act", lambda a: a.copy(out=sq[:, :, cs], in_=pst[:, :].rearrange("p (k t) -> p k t", t=128)), [pst_b], sq_bs)
        for half in range(2):
            wdst, w_b = w_get("mout%d_%d" % (l, half * 512))
            proj_fm(wdst, w_b, 0, 4, sq, sq_bs, KC,
                    lambda mi, pt, pb, half=half: evac(mi, lambda a: a.copy(out=yT[:, half * 4 + mi, :], in_=pt[:, :]),
                                                       lambda v: v.tensor_copy(out=yT[:, half * 4 + mi, :], in_=pt[:, :]), [pb], [yT_b]))

    def kv_stage(seq, ti):
        gblk0 = seq * nblk + ti * NCH
        if ti == 0:
            P.op("dve", lambda v: v.memset(ncprev[:], 0.0), [], [ncprev_b])
        prenorm(16)
        P.op("dve", lambda v: v.memset(kTs[64:65, :, :], 1.0), [], [kTs_b])
        for half in range(2):
            wdst, w_b = w_get("kv_%d" % (half * 512))
            proj_fm(wdst, w_b, 0, 8, hT, hT_bs, KC,
                    lambda h, pt, pb, half=half: evac(h, lambda a: a.copy(out=kTs[0:64, half * 8 + h, :], in_=pt[0:64, :]),
                                                      lambda v: v.tensor_copy(out=kTs[0:64, half * 8 + h, :], in_=pt[0:64, :]), [pb], [kTs_b]), mwidth=64)
        for half in range(2):
            wdst, w_b = w_get("kv_%d" % (1024 + half * 512))
            for c in range(NCH):
                pt, pb = ps_r.next()
                P.mm([(pt[:, :], hT[:, kc, c * 128:(c + 1) * 128], wdst[:, kc, :], kc == 0, kc == KC - 1, [w_b, hT_bs[kc]]) for kc in range(KC)], [pb])
                evac(c + half, lambda a: a.copy(out=vts[:, c, half * 8:(half + 1) * 8, 0:64], in_=pt[:, :].rearrange("p (h d) -> p h d", d=64)),
                     lambda v: v.tensor_copy(out=vts[:, c, half * 8:(half + 1) * 8, 0:64], in_=pt[:, :].rearrange("p (h d) -> p h d", d=64)), [pb], [vts_b])
        wf, wf_b = w_get("kvf")
        G = gsm
        for c in range(NCH):
            cs = slice(c * 128, (c + 1) * 128)
            blk = ti * NCH + c
            pg, pgb = ps_r.next()
            P.mm([(pg[:, 0:16], hT[:, kc, cs], wf[:, kc, :], kc == 0, kc == KC - 1, [wf_b, hT_bs[kc]]) for kc in range(KC)], [pgb])
            P.op("dve", lambda v: v.tensor_tensor(out=G[:, 0, :], in0=pg[:, 0:16], in1=kvb[:, :], op=ALU.add), [pgb], [gsm_b])
            P.op("act", lambda a: a.activation(out=G[:, 1, :], in_=G[:, 0, :], func=AF.Exp, scale=-1.0), [gsm_b], [gsm_b])
            P.op("act", lambda a: a.activation(out=G[:, 1, :], in_=G[:, 1, :], func=AF.Ln, bias=1.0), [gsm_b], [gsm_b])
            pc, pcb = ps_r.next()
            P.mm([(pc[:, 0:16], trif[:, :], G[:, 1, :], True, True, [gsm_b]),
                  (pc[:, 16:32], onesf[:, :], G[:, 1, :], True, True, [gsm_b])], [pcb])
            P.op("dve", lambda v: v.tensor_tensor(out=negc[:, blk, :], in0=pc[:, 0:16], in1=ncprev[:, :], op=ALU.add), [pcb, ncprev_b], [negc_b])
            P.op("dve", lambda v: v.tensor_tensor(out=ncprev[:, :], in0=pc[:, 16:32], in1=ncprev[:, :], op=ALU.add), [pcb], [ncprev_b])
        kvbufs = []
        for c in range(NCH):
            kb_ = Buf("kscr")
            vb_ = Buf("vscr")
            P.dma("sp", kscr_d[gblk0 + c], kTs[0:65, :, c * 128:(c + 1) * 128], [kTs_b], [kb_], kvwpool)
            P.dma("sp", vscr_d[gblk0 + c], vts[:, c, :, :], [vts_b], [vb_], kvwpool)
            kvbufs.append((kb_, vb_))
        return kvbufs

    def fox(j, seq, ti, kvb_all):
        for half in range(2):
            wdst, w_b = w_get("qg%d_%d" % (j, half * 512))
            proj_fm(wdst, w_b, 0, 8, hT, hT_bs, KC,
                    lambda h, pt, pb, half=half: evac(h, lambda a: a.mul(out=QTa[0:64, half * 8 + h, :], in_=pt[0:64, :], mul=0.125),
                                                      lambda v: v.tensor_scalar(out=QTa[0:64, half * 8 + h, :], in0=pt[0:64, :], scalar1=0.125, scalar2=None, op0=ALU.mult),
                                                      [pb], [QTa_b]), mwidth=64)
        for c in range(NCH):
            cs = slice(c * 128, (c + 1) * 128)
            blk = ti * NCH + c
            P.op("dve", lambda v: v.tensor_scalar(out=Caug[:, :, 64], in0=negc[:, blk, :], scalar1=-1.0, scalar2=None, op0=ALU.mult), [negc_b], [Caug_b])
            for hg in range(4):
                pr, prb = ps_r.next()
                P.mm([(pr[0:65, jj * 128:(jj + 1) * 128], Caug[:, hg * 4 + jj, :], ident[:, :], True, True, [Caug_b, ident_b]) for jj in range(4)], [prb])
                evac(hg, lambda a, pr=pr, hg=hg: a.copy(out=QTa[64:65, hg * 4:hg * 4 + 4, cs], in_=pr[64:65, :].rearrange("p (j t) -> p j t", t=128)),
                     lambda v, pr=pr, hg=hg: v.tensor_copy(out=QTa[64:65, hg * 4:hg * 4 + 4, cs], in_=pr[64:65, :].rearrange("p (j t) -> p j t", t=128)),
                     [prb], [QTrow_b])
        for half in range(2):
            wdst, w_b = w_get("qg%d_%d" % (j, 1024 + half * 512))
            proj_fm(wdst, w_b, 0, 8, hT, hT_bs, KC,
                    lambda h, pt, pb, half=half: P.op("act", lambda a: a.activation(out=oT[:, half * 8 + h, :], in_=pt[0:64, :], func=AF.Sigmoid), [pb], [oT_bs[half * 8 + h]]),
                    mwidth=64)
        nkb = ti * NCH + NCH
        GB = 8
        kslots = [(qTf[:].rearrange("p a t -> p (a t)").rearrange("p (b h k) -> p b h k", h=4, k=128), [qT_b]),
                  (kTmf[:].rearrange("p a t -> p (a t)").rearrange("p (b h k) -> p b h k", h=4, k=128), [kTm_b])]
        vslots = [(vtok[:].rearrange("p a t -> p (a t)")[:, 0:GB * 4 * 65].rearrange("p (b h d) -> p b h d", h=4, d=65), vtok_b),
                  (sotok[:].rearrange("p a t -> p (a t)")[:, 0:GB * 4 * 65].rearrange("p (b h d) -> p b h d", h=4, d=65), sotok_b)]
        ps_all = list(ps_r.items)
        slot_i = 0
        for hg in range(4):
            accs = ps_all[0:4]
            ps_r.items = ps_all[4:NPS]
            ps_r.i = 0
            for g0 in range(0, nkb, GB):
                nb = min(GB, nkb - g0)
                ksv, ksbs = kslots[slot_i % 2]
                vsv, vsbs = vslots[slot_i % 2]
                slot_i += 1
                gk0 = seq * nblk + g0
                P.dma("sp", ksv[:, 0:nb], kscr_d[gk0:gk0 + nb, :, hg * 4:hg * 4 + 4, :].rearrange("b p h k -> p b h k"),
                      [kvb_all[g0 + i][0] for i in range(nb)], ksbs, stpool)
                P.dma("sp", vsv[:, 0:nb], vscr_d[gk0:gk0 + nb, :, hg * 4:hg * 4 + 4, :].rearrange("b p h d -> p b h d"),
                      [kvb_all[g0 + i][1] for i in range(nb)], vsbs, stpool)
                pairs = [(kb, hh) for kb in range(g0, g0 + nb) for hh in range(4)]

                def emit_S(kb, hh):
                    ksa = ksv[:, kb - g0]
                    jd = kb - ti * NCH
                    q0 = max(jd, 0) * 128
                    h = hg * 4 + hh
                    pS, pSb = ps_r.next()
                    rds = ksbs + [QTa_b]
                    if jd < 0:
                        items = [(pS[:, 0:T], ksa[:, hh, :], QTa[:, h, 0:T], True, True, rds)]
                    else:
                        items = [(pS[:, q0:q0 + 128], ksa[:, hh, :], QTa[:, h, q0:q0 + 128], True, False, rds),
                                 (pS[:, q0:q0 + 128], ident[:, :], negm[:, :], False, True, [ident_b, const_b])]
                        if q0 + 128 < T:
                            items.append((pS[:, q0 + 128:T], ksa[:, hh, :], QTa[:, h, q0 + 128:T], True, True, rds))
                    P.mm(items, [pSb])
                    return pS, pSb, q0

                def emit_rest(kb, hh, pS, pSb, q0):
                    vsa = vsv[:, kb - g0]
                    h = hg * 4 + hh
                    pta, ptb = pT_r.next()
                    P.op("act", lambda a: a.activation(out=pta[:, q0:T], in_=pS[:, q0:T], func=AF.Exp, bias=negc[:, kb, h:h + 1]),
                         [pSb, negc_b], [ptb])
                    pa, pab = accs[hh]
                    P.mm([(pa[0:65, q0:T], vsa[:, hh, :], pta[:, q0:T], kb == 0, kb == nkb - 1, vsbs + [ptb])], [pab])

                cur = emit_S(*pairs[0])
                for pi in range(len(pairs)):
                    nxt = emit_S(*pairs[pi + 1]) if pi + 1 < len(pairs) else None
                    emit_rest(pairs[pi][0], pairs[pi][1], *cur)
                    cur = nxt
            for hh in range(4):
                h = hg * 4 + hh
                pa, pab = accs[hh]
                P.op("dve", lambda v, pa=pa: v.reciprocal(out=rden[64:65, 0, :], in_=pa[64:65, :]), [pab], [rden_b])
                pb_, pbb = ps_r.next()
                P.mm([(pb_[0:64, :], onesf[64:65, 0:64], rden[64:65, 0, :], True, True, [rden_b])], [pbb])
                P.op("dve", lambda v, h=h, pa=pa: v.tensor_tensor(out=numsb[:, :], in0=pa[0:64, :], in1=oT[:, h, :], op=ALU.mult), [pab, oT_bs[h]], [numsb_b])
                P.op("dve", lambda v, h=h, pb_=pb_: v.tensor_tensor(out=oT[:, h, :], in0=numsb[:, :], in1=pb_[0:64, :], op=ALU.mult), [numsb_b, pbb], [oT_bs[h]])
        ps_r.items = ps_all
        ps_r.i = 0
        for q in range(4):
            wdst, w_b = w_get("fout%d_%d" % (j, q * 256))
            proj_fm(wdst, w_b, 0, 2, oT, oT_bs, 16,
                    lambda mi, pt, pb, q=q: evac(mi, lambda a: a.copy(out=yT[:, q * 2 + mi, :], in_=pt[:, :]),
                                                 lambda v: v.tensor_copy(out=yT[:, q * 2 + mi, :], in_=pt[:, :]), [pb], [yT_b]), npart=64)

    out_evs = []
    for seq in range(nseq):
        kvb_all = []
        for ti in range(ntile):
            t0 = seq * seqlen + ti * T
            P.dma("sp", xT[:], xT_d[:, :, t0:t0 + T], [], xT_bs, xpool)
            if do_kv and 1 not in layers:
                kvb_all.extend(kv_stage(seq, ti))
            for l in layers:
                if ti == 0:
                    for m in range(MFF):
                        pass
                    P.op("dve", lambda v, l=l: v.memset(tails[:, l], 0.0), [], [tails_b[l][m] for m in range(MFF)])
                if "m" in parts:
                    prenorm(l * 4 + 0)
                    if l < 2:
                        mlstm(l, ti == 0)
                    else:
                        fox(l - 2, seq, ti, kvb_all)
                    postnorm_residual(l * 4 + 1)
                if "f" in parts:
                    prenorm(l * 4 + 2)
                    ffn(l)
                    postnorm_residual(l * 4 + 3)
                if l == 1 and do_kv:
                    kvb_all.extend(kv_stage(seq, ti))
            out_evs.append(P.dma("sp", outT_d[:, :, t0:t0 + T], xT[:], xT_bs, [], opool))
    if DEBUG == 2:
        dbg = dict(QTa=(QTa, [yT_b], [65, 16, T], BF16), negc=(negc, [negc_b], [128, nblk, 16], F32), oT=(oT, oT_bs, [64, 16, T], BF16),
                   hT=(hT, hT_bs, [128, KC, T], BF16), pT=(pT, [b for _, b in pT_r.items], [128, NPT, T], BF16), rden=(rden, [rden_b], [65, 1, T], F32),
                   numsb=(numsb, [numsb_b], [64, T], F32))
        dpool = P.pool(1)
        for name, (t_, bufs, shape, dt) in dbg.items():
            dd = nc.dram_tensor("dbg_" + name, shape, dt, kind="ExternalOutput").ap()
            out_evs.append(P.dma("sp", dd, t_[:], bufs, [], dpool))
            P.wait("sp", out_evs[-1])
    if DEBUG == 1:
        dbg = dict(qT=(qT, [qT_b], [64, 8, T], BF16), kTm=(kTm, [kTm_b], [64, 8, T], BF16), vtok=(vtok, vtok_b, [128, NCH, D], BF16),
                   sotok=(sotok, sotok_b, [128, NCH, D], BF16), hstok=(hstok, [hstok_b], [128, D], BF16), hsT=(sq, sq_bs, [128, KC, T], BF16),
                   G=(gsm, [gsm_b], [128, 16, 16], F32), Cst=(Cst, Cst_b[0] + Cst_b[1], [64, 2, 8, 129], F32), hT=(hT, hT_bs, [128, KC, T], BF16),
                   yT=(yT, [yT_b], [128, KC, T], F32), ktok=(ktok, ktok_b, [128, NCH, 512], BF16), Est=(Est, Est_b, [128, 2, 8], F32))
        dpool = P.pool(1)
        for name, (t_, bufs, shape, dt) in dbg.items():
            dd = nc.dram_tensor("dbg_" + name, shape, dt, kind="ExternalOutput").ap()
            out_evs.append(P.dma("sp", dd, t_[:], bufs, [], dpool))
            P.wait("sp", out_evs[-1])
    for ev in out_evs[-3:]:
        P.wait("sp", ev)
    assert wstate["pos"] == len(all_specs), (wstate["pos"], len(all_specs))
    build.stats = dict(cnt=dict(P.cnt), nwait=P.nwait)
    build.es = es
    return nc


def _kmajor(w, kc=None):
    K, N = w.shape
    kc = K // 128
    return np.ascontiguousarray(w.reshape(kc, 128, N).transpose(1, 0, 2))


def prep_shared(inp):
    f = np.float32
    sh = {}
    norms = np.zeros((17, D), f)
    for l in range(4):
        norms[l * 4 + 0] = inp["norm_mix_pre"][l]
        norms[l * 4 + 1] = inp["norm_mix_post"][l]
        norms[l * 4 + 2] = inp["norm_ffn_pre"][l]
        norms[l * 4 + 3] = inp["norm_ffn_post"][l]
    norms[16] = inp["kv_norm"]
    sh["norms"] = np.ascontiguousarray(norms.reshape(17, KC, 128).transpose(2, 0, 1))
    for l in range(2):
        sh["w_in%d" % l] = _kmajor(inp["mlstm_w_in"][l])
        sh["w_mout%d" % l] = _kmajor(inp["mlstm_w_out"][l])
        sh["w_qg%d" % l] = _kmajor(inp["fox_w_qg"][l])
        sh["w_fout%d" % l] = np.ascontiguousarray(inp["fox_w_out"][l].reshape(16, 64, D).transpose(1, 0, 2))
    sh["bgate"] = np.ascontiguousarray(np.broadcast_to(inp["mlstm_b_gate"][None], (128, 2, 16))).astype(f)
    sh["hnorm"] = np.ascontiguousarray(np.broadcast_to(inp["mlstm_norm"][None], (128, 2, D))).astype(f)
    sh["kvw"] = _kmajor(inp["kv_w"])
    sh["kvb"] = np.ascontiguousarray(np.broadcast_to(inp["kv_b_f"][None], (128, 16))).astype(f)
    for l in range(4):
        sh["w_up%d" % l] = _kmajor(inp["ffn_w_up"][l])
        sh["w_dn%d" % l] = _kmajor(inp["ffn_w_down"][l])
    cw = np.zeros((4, 4, 2 * DFF), f)
    cw[:, 0:3] = inp["ffn_conv_w"]
    cw[:, 3] = inp["ffn_conv_b"]
    sh["convw"] = np.ascontiguousarray(cw.reshape(4, 4, MFF, 128).transpose(3, 0, 2, 1))
    sh["ident"] = np.eye(128, dtype=f)
    s = np.arange(128)
    sh["tri"] = (s[:, None] <= s[None, :]).astype(f)
    sh["negm"] = np.where(s[:, None] <= s[None, :], 0.0, NEG).astype(f)
    return sh


def x_to_core(xs):
    n, S, _ = xs.shape
    return np.ascontiguousarray(xs.reshape(n * S, KC, 128).transpose(2, 1, 0))


def core_to_x(o, nseq, S):
    return np.ascontiguousarray(o.transpose(2, 1, 0).reshape(nseq, S, D))


def kernel(**inputs):
    inp = {k: np.asarray(v, dtype=np.float32) for k, v in inputs.items()}
    x = inp["x"]
    B, S, _ = x.shape
    ncores = 8
    nseq = B // ncores
    nc = build(nseq, S)
    sh = prep_shared(inp)
    in_maps = []
    for c in range(ncores):
        m = dict(sh)
        m["xT"] = x_to_core(x[c * nseq:(c + 1) * nseq])
        in_maps.append(m)
    res = run_bass_kernel_spmd(nc, in_maps, core_ids=list(range(ncores)))
    out = np.empty_like(x)
    for c in range(ncores):
        out[c * nseq:(c + 1) * nseq] = core_to_x(res.results[c]["outT"], nseq, S)
    return out
```

```python
from contextlib import ExitStack
import numpy as np
import ml_dtypes
import concourse.bass as bass
import concourse.mybir as mybir
from concourse.bass_utils import run_bass_kernel_spmd

F32 = mybir.dt.float32
BF16 = mybir.dt.bfloat16
AF = mybir.ActivationFunctionType
ALU = mybir.AluOpType
AX = mybir.AxisListType

D = 1024
KC = 8
T = 512
NCH = T // 128
DFF = 2816
MFF = 44
EPS = 1e-6
NLAYER = 4
MIN = 3088
NEG = -30000.0
DEBUG = False


class Buf:
    __slots__ = ("name", "w", "r", "excl")

    def __init__(self, name, excl=False):
        self.name = name
        self.w = None
        self.r = {}
        self.excl = excl


class SemPool:
    def __init__(self, sems):
        self.sems = sems
        self.tot = [0] * len(sems)
        self.i = 0

    def next(self):
        i = self.i
        self.i = (i + 1) % len(self.sems)
        return i


class Prog:
    def __init__(self, nc, es):
        self.nc = nc
        self.eng = {"pe": nc.tensor, "act": nc.scalar, "dve": nc.vector, "pool": nc.gpsimd, "sp": nc.sync}
        self.sem = {}
        self.cnt = {}
        self.seen = {}
        self.semobj = {}
        for e in self.eng:
            s = es.enter_context(nc.semaphore("sem_" + e))
            self.semobj["E" + e] = s
            self.cnt[e] = 0
            self.seen[e] = {}
        self.es = es
        self.npool = 0
        self.nwait = 0

    def pool(self, n):
        sems = []
        for i in range(n):
            key = "P%d_%d" % (self.npool, i)
            s = self.es.enter_context(self.nc.semaphore(key))
            self.semobj[key] = s
            sems.append(key)
        self.npool += 1
        return SemPool(sems)

    def wait(self, e, ev):
        if ev is None:
            return
        key, val = ev
        if val <= 0:
            return
        if e == "pe" and key == "Epe":
            return
        if self.seen[e].get(key, 0) >= val:
            return
        self.eng[e].wait_ge(self.semobj[key], val)
        self.seen[e][key] = val
        self.nwait += 1

    def deps(self, e, reads, writes):
        for b in reads:
            self.wait(e, b.w)
            if b.excl:
                for k, v in b.r.items():
                    self.wait(e, (k, v))
        for b in writes:
            self.wait(e, b.w)
            for k, v in b.r.items():
                self.wait(e, (k, v))

    def mark(self, ev, reads, writes):
        for b in reads:
            b.r[ev[0]] = ev[1]
        for b in writes:
            b.w = ev
            b.r = {}

    def op(self, e, fn, reads=(), writes=()):
        self.deps(e, reads, writes)
        ins = fn(self.eng[e])
        self.cnt[e] += 1
        ins.then_inc(self.semobj["E" + e], 1)
        self.mark(("E" + e, self.cnt[e]), reads, writes)

    def mm(self, items, writes):
        ev = ("Epe", self.cnt["pe"] + 1)
        ins = None
        allr = []
        for (out, lhsT, rhs, st, sp, reads) in items:
            self.deps("pe", reads, writes)
            ins = self.nc.tensor.matmul(out, lhsT, rhs, start=st, stop=sp)
            for b in reads:
                if b not in allr:
                    allr.append(b)
        self.cnt["pe"] += 1
        ins.then_inc(self.semobj["Epe"], 1)
        self.mark(ev, allr, writes)

    def transposes(self, items, writes):
        ev = ("Epe", self.cnt["pe"] + 1)
        ins = None
        allr = []
        for (out, in_, ident, reads) in items:
            self.deps("pe", reads, writes)
            ins = self.nc.tensor.transpose(out, in_, ident)
            for b in reads:
                if b not in allr:
                    allr.append(b)
        self.cnt["pe"] += 1
        ins.then_inc(self.semobj["Epe"], 1)
        self.mark(ev, allr, writes)

    def dma(self, q, out, in_, reads, writes, pool):
        i = pool.next()
        key = pool.sems[i]
        self.wait(q, (key, pool.tot[i]))
        self.deps(q, reads, writes)
        ins = self.eng[q].dma_start(out=out, in_=in_)
        ins.then_inc(self.semobj[key], 16)
        pool.tot[i] += 16
        ev = (key, pool.tot[i])
        self.mark(ev, reads, writes)
        return ev


class Rot:
    def __init__(self, items):
        self.items = items
        self.i = 0

    def next(self):
        it = self.items[self.i]
        self.i = (self.i + 1) % len(self.items)
        return it


def build(nseq, seqlen, layers=(0, 1, 2, 3), do_kv=True, parts="mf"):
    assert seqlen % T == 0
    ntile = seqlen // T
    nblk = seqlen // 128
    ntok = nseq * seqlen
    nc = bass.Bass("TRN2", target_bir_lowering=False)
    es = ExitStack()
    P = Prog(nc, es)

    def dram_in(name, shape, dt=F32):
        return nc.dram_tensor(name, list(shape), dt, kind="ExternalInput").ap()

    xT_d = dram_in("xT", [128, KC, ntok])
    outT_d = nc.dram_tensor("outT", [128, KC, ntok], F32, kind="ExternalOutput").ap()
    norms_d = dram_in("norms", [128, 17, KC])
    w_in_d = [dram_in("w_in%d" % l, [128, KC, MIN]) for l in range(2)]
    w_mout_d = [dram_in("w_mout%d" % l, [128, KC, D]) for l in range(2)]
    bgate_d = dram_in("bgate", [128, 2, 16])
    hnorm_d = dram_in("hnorm", [128, 2, D])
    kvw_d = dram_in("kvw", [128, KC, 2064])
    kvb_d = dram_in("kvb", [128, 16])
    w_qg_d = [dram_in("w_qg%d" % l, [128, KC, 2 * D]) for l in range(2)]
    w_fout_d = [dram_in("w_fout%d" % l, [64, 16, D]) for l in range(2)]
    w_up_d = [dram_in("w_up%d" % l, [128, KC, 2 * DFF]) for l in range(4)]
    w_dn_d = [dram_in("w_dn%d" % l, [128, 22, D]) for l in range(4)]
    convw_d = dram_in("convw", [128, 4, MFF, 4])
    ident_d = dram_in("ident", [128, 128])
    tri_d = dram_in("tri", [128, 128])
    negm_d = dram_in("negm", [128, 128])
    kscr_d = nc.dram_tensor("kscr", [nseq * nblk, 65, 16, 128], BF16, kind="Internal").ap()
    vscr_d = nc.dram_tensor("vscr", [nseq * nblk, 128, 16, 65], BF16, kind="Internal").ap()

    def sb(name, shape, dt):
        return es.enter_context(nc.sbuf_tensor(name, list(shape), dt))

    xT = sb("xT_s", [128, KC, T], F32); xT_bs = [Buf("xT%d" % i) for i in range(KC)]
    hT = sb("hT_s", [128, KC, T], BF16); hT_bs = [Buf("hT%d" % i) for i in range(KC)]
    yT = sb("yT_s", [128, KC, T], F32); yT_b = Buf("yT")
    sq = sb("sq_s", [128, KC, T], BF16); sq_bs = [Buf("sq%d" % i) for i in range(KC)]
    rstd = sb("rstd_s", [128, T], F32); rstd_b = Buf("rstd")
    lnt = rstd; lnt_b = rstd_b
    norms = sb("norms_s", [128, 17, KC], F32); norms_b = Buf("norms")
    gT = sb("gT_s", [128, 22, T], BF16); gT_bs = [Buf("gT%d" % i) for i in range(22)]
    convw = sb("convw_s", [128, 4, MFF, 4], F32); convw_b = Buf("convw")
    tails = sb("tails_s", [128, 4, MFF, 2], F32); tails_b = [[Buf("tail%d_%d" % (l, m)) for m in range(MFF)] for l in range(4)]
    NUE = 4
    ue = sb("ue_s", [128, NUE, T + 2], F32); ue_r = Rot([(ue[:, i, :], Buf("ue%d" % i)) for i in range(NUE)])
    yc = sb("yc_s", [128, NUE, T], F32); yc_r = Rot([(yc[:, i, :], Buf("yc%d" % i)) for i in range(NUE)])
    tmpn = yc; tmpn_bs = [yc_r.items[0][1], yc_r.items[1][1]]
    NWB = 3
    wb = sb("wb_s", [128, NWB, 4096], BF16); wb_r = Rot([(wb[:, i, :], Buf("wb%d" % i)) for i in range(NWB)])
    wsm = sb("wsm_s", [128, 2, KC, 16], BF16); wsm_r = Rot([(wsm[:, i], Buf("wsm%d" % i)) for i in range(2)])
    ident = sb("ident_s", [128, 128], BF16); ident_b = Buf("ident")
    trif = sb("trif_s", [128, 128], F32); onesf = sb("onesf_s", [128, 128], F32)
    onesb = sb("onesb_s", [128, 128], BF16)
    mask4 = sb("mask4_s", [128, 4, 128], BF16)
    negm = sb("negm_s", [128, 128], BF16)
    const_b = Buf("consts")
    qTf = sb("qT_s", [65, 8, T], BF16); qT = qTf[0:64]; qT_b = Buf("qT")
    kTmf = sb("kTm_s", [65, 8, T], BF16); kTm = kTmf[0:64]; kTm_b = Buf("kTm")
    ktok = sb("ktok_s", [128, NCH, 512], BF16); ktok_b = [Buf("ktok%d" % c) for c in range(NCH)]
    vtok = sb("vtok_s", [128, NCH, D], BF16); vtok_b = [Buf("vtok%d" % c) for c in range(NCH)]
    sotok = sb("sotok_s", [128, NCH, D], BF16); sotok_b = [Buf("sotok%d" % c) for c in range(NCH)]
    bgate = sb("bgate_s", [128, 2, 16], F32)
    hnorm = sb("hnorm_s", [128, 2, D], F32)
    Cst = sb("Cst_s", [64, 2, 8, 129], F32); Cst_b = [[Buf("C%d_%d" % (l, h)) for h in range(8)] for l in range(2)]
    Cw = sb("Cw_s", [64, 8, 129], BF16); Cw_b = [Buf("Cw%d" % h) for h in range(8)]
    Est = sb("Est_s", [128, 2, 8], F32); Est_b = [Buf("E%d" % l) for l in range(2)]
    gsm = sb("gsm_s", [128, 16, 16], F32); gsm_b = Buf("gsm"); gW_b = Buf("gW"); gOut_b = Buf("gOut")
    vext = sb("vext_s", [128, 1, 8, 129], BF16); vext_r = Rot([(vext[:, i], [Buf("vext%d_%d" % (i, h)) for h in range(8)]) for i in range(1)])
    stm = sb("stm_s", [128, 2, 4, 128], BF16); stm_r = Rot([(stm[:, i], Buf("stm%d" % i)) for i in range(2)])
    gw = sb("gw_s", [128, D], F32); gw_b = Buf("gw")
    hstok = sb("hstok_s", [128, D], BF16); hstok_b = Buf("hstok")
    junk = sb("junk_s", [128, 128], F32); junk_b = Buf("junk")
    y16 = yT[:].bitcast(BF16).rearrange("p k (a t) -> p (k a) t", t=T)
    print("y16", y16.shape)
    kTs = y16[0:65]; kTs_b = yT_b
    vts = sb("vts_s", [128, NCH, 16, 65], BF16); vts_b = Buf("vts")
    kvb = sb("kvb_s", [128, 16], F32)
    negc = sb("negc_s", [128, nblk, 16], F32); negc_b = Buf("negc")
    ncprev = sb("ncprev_s", [128, 16], F32); ncprev_b = Buf("ncprev")
    Caug = sb("Caug_s", [128, 16, 65], BF16); Caug_b = Buf("Caug")
    QTa = y16[0:65]; QTa_b = yT_b; QTrow_b = yT_b
    oT = gT[0:64, 0:16, :]; oT_bs = gT_bs[0:16]
    NST = 1
    NPT = 2
    pT = sb("pT_s", [128, NPT, T], BF16); pT_r = Rot([(pT[:, i, :], Buf("pT%d" % i)) for i in range(NPT)])
    rden = sb("rden_s", [65, 1, T], F32); rden_b = Buf("rden")
    rhl = sb("rhl_s", [65, 2, T], BF16); rhl_b = Buf("rhl")
    numsb = sb("numsb_s", [64, T], F32); numsb_b = Buf("numsb")

    NPS = 7
    ps_r = Rot([])
    for i in range(NPS):
        pt = es.enter_context(nc.psum_tensor("ps%d" % i, [128, 512], F32))
        ps_r.items.append((pt, Buf("ps%d" % i, excl=True)))
    pst = es.enter_context(nc.psum_tensor("pst", [128, 1024], BF16)); pst_b = Buf("pst", excl=True)

    wpool = P.pool(8)
    cpool = P.pool(4)
    xpool = P.pool(2)
    opool = P.pool(2)
    kvwpool = P.pool(8)
    stpool = P.pool(8)

    cev = []
    cev.append(P.dma("sp", norms[:], norms_d, [], [norms_b], cpool))
    cev.append(P.dma("sp", convw[:], convw_d, [], [convw_b], cpool))
    cev.append(P.dma("sp", trif[:], tri_d, [], [const_b], cpool))
    cev.append(P.dma("sp", bgate[:], bgate_d, [], [const_b], cpool))
    cev.append(P.dma("sp", hnorm[:], hnorm_d, [], [const_b], cpool))
    cev.append(P.dma("sp", kvb[:], kvb_d, [], [const_b], cpool))
    cev.append(P.dma("pool", negm[:], negm_d, [], [const_b], cpool))
    cev.append(P.dma("pool", ident[:], ident_d, [], [ident_b], cpool))
    for e in ("pe", "act", "dve"):
        for ev in cev:
            P.wait(e, ev)
    P.op("dve", lambda v: v.memset(onesf[:], 1.0), [], [const_b])
    P.op("dve", lambda v: v.memset(onesb[:], 1.0), [], [const_b])
    for j in range(4):
        P.op("dve", lambda v, j=j: v.tensor_copy(out=mask4[:, j, :], in_=trif[:]), [], [const_b])
    P.op("dve", lambda v: v.memset(Caug[:], 0.0), [], [Caug_b])
    P.op("dve", lambda v: v.memset(vts[:], 1.0), [], [vts_b])

    prepool = P.pool(16)
    wsrc = {}

    def convert(name, src, shape):
        dst = nc.dram_tensor(name + "_bf", list(shape), BF16, kind="Internal").ap()
        rows = shape[0] * shape[1]
        s2 = src.rearrange("p k n -> (p k) n")
        d2 = dst.rearrange("p k n -> (p k) n")
        bufs = []
        R = 32
        for r0 in range(0, rows, R):
            b = Buf(name + "_c%d" % r0)
            r1 = min(rows, r0 + R)
            P.dma("pool", d2[r0:r1, :], s2[r0:r1, :], [], [b], prepool)
            bufs.append(b)
        wsrc[name] = (dst, bufs)

    for l in layers:
        if "m" in parts and l < 2:
            convert("w_in%d" % l, w_in_d[l], [128, KC, MIN])
            convert("w_mout%d" % l, w_mout_d[l], [128, KC, D])
        if "m" in parts and l >= 2:
            if "kvw" not in wsrc and do_kv:
                convert("kvw", kvw_d, [128, KC, 2064])
            convert("w_qg%d" % (l - 2), w_qg_d[l - 2], [128, KC, 2 * D])
            convert("w_fout%d" % (l - 2), w_fout_d[l - 2], [64, 16, D])
        if "f" in parts:
            convert("w_up%d" % l, w_up_d[l], [128, KC, 2 * DFF])
            convert("w_dn%d" % l, w_dn_d[l], [128, 22, D])
        if l == 1 and do_kv and "kvw" not in wsrc:
            convert("kvw", kvw_d, [128, KC, 2064])

    def WS(name):
        return wsrc[name]

    wstate = {"pending": [], "specs": [], "pos": 0, "issued": 0}

    def wspecs_for_tile():
        specs = []

        def add(tag, name, c0, n, npart, k):
            dst, bufs = WS(name)
            specs.append((tag, dst[:, :, c0:c0 + n], npart, k, n, bufs))

        def add_kv():
            for c0 in range(0, 2048, 512):
                add("kv_%d" % c0, "kvw", c0, 512, 128, KC)
            add("kvf", "kvw", 2048, 16, 128, KC)

        if do_kv and 1 not in layers:
            add_kv()
        for l in layers:
            if "m" not in parts:
                pass
            elif l < 2:
                for c0 in range(0, 3072, 512):
                    add("min%d_%d" % (l, c0), "w_in%d" % l, c0, 512, 128, KC)
                add("mif%d" % l, "w_in%d" % l, 3072, 16, 128, KC)
                for c0 in (0, 512):
                    add("mout%d_%d" % (l, c0), "w_mout%d" % l, c0, 512, 128, KC)
            else:
                j = l - 2
                for c0 in range(0, 2048, 512):
                    add("qg%d_%d" % (j, c0), "w_qg%d" % j, c0, 512, 128, KC)
                for c0 in range(0, 1024, 256):
                    add("fout%d_%d" % (j, c0), "w_fout%d" % j, c0, 256, 64, 16)
            if "f" in parts:
                for c0 in range(0, 2 * DFF, 512):
                    add("up%d_%d" % (l, c0), "w_up%d" % l, c0, 512, 128, KC)
                for c0 in range(0, D, 128):
                    add("dn%d_%d" % (l, c0), "w_dn%d" % l, c0, 128, 128, 22)
            if l == 1 and do_kv:
                add_kv()
        return specs

    tile_specs = wspecs_for_tile()
    all_specs = tile_specs * (nseq * ntile)
    LA = 2

    def w_issue():
        i = wstate["issued"]
        if i >= len(all_specs):
            return
        tag, src, npart, k, n, rbufs = all_specs[i]
        if n == 16:
            ap, b = wsm_r.next()
            dst = ap[:, :, :]
        else:
            ap, b = wb_r.next()
            dst = ap[0:npart, 0:k * n].rearrange("p (k n) -> p k n", n=n)
        P.dma("pool", dst, src, rbufs, [b], wpool)
        wstate["pending"].append((tag, dst, b))
        wstate["issued"] = i + 1

    def w_get(tag):
        while wstate["issued"] < min(len(all_specs), wstate["pos"] + LA + 1):
            w_issue()
        t, dst, b = wstate["pending"].pop(0)
        assert t == tag, (t, tag)
        wstate["pos"] += 1
        return dst, b

    def evac(i, fn_act, fn_dve, reads, writes):
        if i % 2 == 0:
            P.op("act", fn_act, reads, writes)
        else:
            P.op("dve", fn_dve, reads, writes)

    def rmsnorm_stats(src, src_bs, nidx):
        pt, pb = ps_r.next()
        for kc in range(KC):
            sb_ = src_bs[kc] if isinstance(src_bs, list) else src_bs
            P.op("act", lambda a, kc=kc: a.activation(out=sq[:, kc, :], in_=src[:, kc, :], func=AF.Square), [sb_], [sq_bs[kc]])
        P.mm([(pt[:, :], onesb[:, :], sq[:, kc, :], kc == 0, kc == KC - 1, [sq_bs[kc]]) for kc in range(KC)], [pb])
        P.op("act", lambda a: a.activation(out=lnt[:], in_=pt[:, :], func=AF.Ln, scale=1.0 / D, bias=EPS), [pb], [lnt_b])
        P.op("act", lambda a: a.activation(out=rstd[:], in_=lnt[:], func=AF.Exp, scale=-0.5), [lnt_b], [rstd_b])

    def prenorm(nidx):
        rmsnorm_stats(xT, xT_bs, nidx)
        for kc in range(KC):
            P.op("dve", lambda v, kc=kc: v.scalar_tensor_tensor(out=hT[:, kc, :], in0=xT[:, kc, :], scalar=norms[:, nidx, kc:kc + 1],
                                                             in1=rstd[:], op0=ALU.mult, op1=ALU.mult),
                 [xT_bs[kc], rstd_b, norms_b], [hT_bs[kc]])

    def postnorm_residual(nidx):
        rmsnorm_stats(yT, yT_b, nidx)
        for kc in range(KC):
            tb = tmpn_bs[kc % 2]
            P.op("dve", lambda v, kc=kc: v.tensor_tensor(out=tmpn[:, kc % 2, :], in0=yT[:, kc, :], in1=rstd[:], op=ALU.mult),
                 [yT_b, rstd_b], [tb])
            P.op("dve", lambda v, kc=kc: v.scalar_tensor_tensor(out=xT[:, kc, :], in0=tmpn[:, kc % 2, :], scalar=norms[:, nidx, kc:kc + 1],
                                                                 in1=xT[:, kc, :], op0=ALU.mult, op1=ALU.add),
                 [tb, norms_b], [xT_bs[kc]])

    def proj_fm(wdst, w_b, m_lo, m_n, src, src_b, kcn, out_fn, npart=128, mwidth=128):
        for mi in range(m_n):
            pt, pb = ps_r.next()
            c0 = m_lo + mi * mwidth
            P.mm([(pt[0:mwidth, :], wdst[0:npart, kc, c0:c0 + mwidth], src[0:npart, kc, :], kc == 0, kc == kcn - 1,
                   [w_b, (src_b[kc] if isinstance(src_b, list) else src_b)])
                  for kc in range(kcn)], [pb])
            out_fn(mi, pt, pb)

    def ffn(l):
        pending = [None]
        for blk in range(11):
            wdst, w_b = w_get("up%d_%d" % (l, blk * 512))
            for mi in range(4):
                m = blk * 4 + mi
                pt, pb = ps_r.next()
                P.mm([(pt[:, :], wdst[:, kc, mi * 128:(mi + 1) * 128], hT[:, kc, :], kc == 0, kc == KC - 1, [w_b, hT_bs[kc]])
                      for kc in range(KC)], [pb])
                uea, ueb = ue_r.next()
                yca, ycb = yc_r.next()
                tb = tails_b[l][m]
                P.op("dve", lambda v: v.tensor_copy(out=uea[:, 0:2], in_=tails[:, l, m, :]), [tb], [ueb])
                P.op("act", lambda a: a.copy(out=uea[:, 2:T + 2], in_=pt[:, :]), [pb], [ueb])
                P.op("act", lambda a: a.activation(out=yca, in_=uea[:, 2:T + 2], func=AF.Identity, scale=convw[:, l, m, 2:3],
                                                   bias=convw[:, l, m, 3:4]), [ueb, convw_b], [ycb])

                def stage_b(m=m, uea=uea, ueb=ueb, yca=yca, ycb=ycb):
                    P.op("dve", lambda v: v.scalar_tensor_tensor(out=yca, in0=uea[:, 1:T + 1], scalar=convw[:, l, m, 1:2], in1=yca,
                                                                 op0=ALU.mult, op1=ALU.add), [ueb, convw_b, ycb], [ycb])
                    P.op("dve", lambda v: v.scalar_tensor_tensor(out=yca, in0=uea[:, 0:T], scalar=convw[:, l, m, 0:1], in1=yca,
                                                                 op0=ALU.mult, op1=ALU.add), [ueb, convw_b, ycb], [ycb])
                    if m < 22:
                        P.op("act", lambda a: a.activation(out=gT[:, m, :], in_=yca, func=AF.Gelu_apprx_tanh), [ycb], [gT_bs[m]])
                    else:
                        mm_ = m - 22
                        P.op("dve", lambda v: v.tensor_tensor(out=gT[:, mm_, :], in0=gT[:, mm_, :], in1=yca, op=ALU.mult),
                             [ycb], [gT_bs[mm_]])

                if pending[0] is not None:
                    pending[0]()
                pending[0] = stage_b
                P.op("dve", lambda v: v.tensor_copy(out=tails[:, l, m, :], in_=uea[:, T:T + 2]), [ueb], [tb])
        pending[0]()
        for mo in range(8):
            wdst, w_b = w_get("dn%d_%d" % (l, mo * 128))
            pt, pb = ps_r.next()
            P.mm([(pt[:, :], wdst[:, kc, :], gT[:, kc, :], kc == 0, kc == 21, [w_b, gT_bs[kc]]) for kc in range(22)], [pb])
            evac(mo, lambda a: a.copy(out=yT[:, mo, :], in_=pt[:, :]), lambda v: v.tensor_copy(out=yT[:, mo, :], in_=pt[:, :]), [pb], [yT_b])

    def mlstm(l, first_tile):
        if first_tile:
            P.op("dve", lambda v: v.memset(Cst[:, l], 0.0), [], Cst_b[l])
            P.op("dve", lambda v: v.memset(Est[:, l, :], 1.0), [], [Est_b[l]])
        wdst, w_b = w_get("min%d_%d" % (l, 0))
        proj_fm(wdst, w_b, 0, 8, hT, hT_bs, KC,
                lambda h, pt, pb: evac(h, lambda a: a.mul(out=qT[:, h, :], in_=pt[0:64, :], mul=0.125),
                                       lambda v: v.tensor_scalar(out=qT[:, h, :], in0=pt[0:64, :], scalar1=0.125, scalar2=None, op0=ALU.mult),
                                       [pb], [qT_b]), mwidth=64)
        wdst, w_b = w_get("min%d_%d" % (l, 512))
        proj_fm(wdst, w_b, 0, 8, hT, hT_bs, KC,
                lambda h, pt, pb: evac(h + 1, lambda a: a.copy(out=kTm[:, h, :], in_=pt[0:64, :]),
                                       lambda v: v.tensor_copy(out=kTm[:, h, :], in_=pt[0:64, :]), [pb], [kTm_b]), mwidth=64)
        for c in range(NCH):
            pt, pb = ps_r.next()
            P.mm([(pt[:, :], hT[:, kc, c * 128:(c + 1) * 128], wdst[:, kc, :], kc == 0, kc == KC - 1, [w_b, hT_bs[kc]]) for kc in range(KC)], [pb])
            evac(c, lambda a: a.copy(out=ktok[:, c, :], in_=pt[:, :]), lambda v: v.tensor_copy(out=ktok[:, c, :], in_=pt[:, :]), [pb], [ktok_b[c]])
        for half in range(2):
            wdst, w_b = w_get("min%d_%d" % (l, 1024 + half * 512))
            for c in range(NCH):
                pt, pb = ps_r.next()
                P.mm([(pt[:, :], hT[:, kc, c * 128:(c + 1) * 128], wdst[:, kc, :], kc == 0, kc == KC - 1, [w_b, hT_bs[kc]]) for kc in range(KC)], [pb])
                evac(c + half, lambda a: a.copy(out=vtok[:, c, half * 512:(half + 1) * 512], in_=pt[:, :]),
                     lambda v: v.tensor_copy(out=vtok[:, c, half * 512:(half + 1) * 512], in_=pt[:, :]), [pb], [vtok_b[c]])
        for half in range(2):
            wdst, w_b = w_get("min%d_%d" % (l, 2048 + half * 512))
            for c in range(NCH):
                pt, pb = ps_r.next()
                P.mm([(pt[:, :], hT[:, kc, c * 128:(c + 1) * 128], wdst[:, kc, :], kc == 0, kc == KC - 1, [w_b, hT_bs[kc]]) for kc in range(KC)], [pb])
                P.op("act", lambda a: a.activation(out=sotok[:, c, half * 512:(half + 1) * 512], in_=pt[:, :], func=AF.Sigmoid), [pb], [sotok_b[c]])
        wif, wif_b = w_get("mif%d" % l)
        G = gsm
        for c in range(NCH):
            cs = slice(c * 128, (c + 1) * 128)
            pg, pgb = ps_r.next()
            P.mm([(pg[:, 0:16], hT[:, kc, cs], wif[:, kc, :], kc == 0, kc == KC - 1, [wif_b, hT_bs[kc]]) for kc in range(KC)], [pgb])
            P.op("dve", lambda v: v.tensor_tensor(out=G[:, 0, :], in0=pg[:, 0:16], in1=bgate[:, l, :], op=ALU.add), [pgb], [gsm_b])
            P.op("act", lambda a: a.activation(out=G[:, 1, 0:8], in_=G[:, 0, 8:16], func=AF.Exp, scale=-1.0), [gsm_b], [gsm_b])
            P.op("act", lambda a: a.activation(out=G[:, 1, 0:8], in_=G[:, 1, 0:8], func=AF.Ln, bias=1.0), [gsm_b], [gsm_b])
            pc, pcb = ps_r.next()
            P.mm([(pc[:, 0:8], trif[:, :], G[:, 1, 0:8], True, True, [gsm_b]),
                  (pc[:, 8:16], onesf[:, :], G[:, 1, 0:8], True, True, [gsm_b])], [pcb])
            P.op("dve", lambda v: v.tensor_tensor(out=G[:, 2, 0:8], in0=pc[:, 0:8], in1=G[:, 0, 0:8], op=ALU.add), [pcb, gsm_b], [gsm_b])
            P.op("act", lambda a: a.activation(out=G[:, 1, 8:16], in_=G[:, 2, 0:8], func=AF.Exp), [gsm_b], [gsm_b])
            P.op("act", lambda a: a.activation(out=G[:, 5, 0:8], in_=pc[:, 8:16], func=AF.Exp, scale=-1.0), [pcb], [gsm_b])
            P.op("act", lambda a: a.activation(out=G[:, 5, 8:16], in_=pc[:, 0:8], func=AF.Exp), [pcb], [gsm_b])
            pe_, peb = ps_r.next()
            P.mm([(pe_[:, 0:8], onesf[:, :], G[:, 1, 8:16], True, True, [gsm_b])], [peb])
            P.op("dve", lambda v: v.tensor_tensor(out=G[:, 3, 0:8], in0=pe_[:, 0:8], in1=Est[:, l, :], op=ALU.add), [peb, Est_b[l]], [gsm_b])
            P.op("dve", lambda v: v.reciprocal(out=G[:, 3, 8:16], in_=G[:, 3, 0:8]), [gsm_b], [gsm_b])
            P.op("dve", lambda v: v.tensor_tensor(out=G[:, 4, 0:8], in0=G[:, 1, 8:16], in1=G[:, 3, 8:16], op=ALU.mult), [gsm_b], [gW_b])
            P.op("dve", lambda v: v.tensor_tensor(out=G[:, 4, 8:16], in0=Est[:, l, :], in1=G[:, 3, 8:16], op=ALU.mult), [gsm_b, Est_b[l], gW_b], [gW_b])
            P.op("dve", lambda v: v.tensor_tensor(out=Est[:, l, :], in0=G[:, 5, 0:8], in1=G[:, 3, 0:8], op=ALU.mult), [gsm_b], [Est_b[l]])
            P.op("dve", lambda v: v.tensor_tensor(out=G[:, 6, 0:8], in0=G[:, 5, 8:16], in1=G[:, 3, 8:16], op=ALU.mult), [gsm_b], [gsm_b])
            for h in range(8):
                P.op("act", lambda a, h=h: a.activation(out=Cw[:, h, :], in_=Cst[:, l, h, :], func=AF.Identity, scale=G[0:64, 4, 8 + h:9 + h]),
                     [Cst_b[l][h], gW_b], [Cw_b[h]])
            vxa, vxb = vext_r.next()
            for h in range(8):
                P.op("act", lambda a, h=h: a.activation(out=vxa[:, h, 0:128], in_=vtok[:, c, h * 128:(h + 1) * 128], func=AF.Identity,
                                                        scale=G[:, 4, h:h + 1]), [vtok_b[c], gW_b], [vxb[h]])
            P.op("dve", lambda v: v.tensor_copy(out=vxa[:, :, 128], in_=G[:, 4, 0:8]), [gW_b], vxb)
            sts = []
            for hg in range(2):
                pS, pSb = ps_r.next()
                P.mm([(pS[:, j * 128:(j + 1) * 128], kTm[:, hg * 4 + j, cs], qT[:, hg * 4 + j, cs], True, True, [kTm_b, qT_b]) for j in range(4)], [pSb])
                sta, stb = stm_r.next()
                P.op("dve", lambda v, sta=sta, pS=pS: v.tensor_tensor(out=sta[:, :, :], in0=pS[:, :].rearrange("p (j t) -> p j t", t=128), in1=mask4[:, :, :], op=ALU.mult),
                     [pSb, const_b], [stb])
                sts.append((sta, stb))
            nums = {}
            for grp in ((0, 1, 2), (3, 4, 5), (6, 7)):
                pn, pnb = ps_r.next()
                items = []
                for j, h in enumerate(grp):
                    sta, stb = sts[h // 4]
                    o_ = pn[:, j * 129:(j + 1) * 129]
                    items.append((o_, sta[:, h % 4, :], vxa[:, h, :], True, False, [stb, vxb[h]]))
                    items.append((o_, qT[:, h, cs], Cw[:, h, :], False, True, [qT_b, Cw_b[h]]))
                    nums[h] = (pn, pnb, j)
                P.mm(items, [pnb])
            for hg in range(3):
                grp = ((0, 1, 2), (3, 4, 5), (6, 7))[hg]
                pd, pdb = ps_r.next()
                P.mm([(pd[0:64, j * 129:(j + 1) * 129], ktok[:, c, h * 64:(h + 1) * 64], vxa[:, h, :], True, True, [ktok_b[c], vxb[h]]) for j, h in enumerate(grp)], [pdb])
                for j, h in enumerate(grp):
                    P.op("dve", lambda v, j=j, h=h, pd=pd: v.scalar_tensor_tensor(out=Cst[:, l, h, :], in0=Cst[:, l, h, :], scalar=G[0:64, 4, 8 + h:9 + h],
                                                                                   in1=pd[0:64, j * 129:(j + 1) * 129], op0=ALU.mult, op1=ALU.add),
                         [pdb, gW_b, Cw_b[h]], [Cst_b[l][h]])
            for h in range(8):
                pn, pnb, j = nums[h]
                P.op("act", lambda a, h=h, pn=pn, j=j: a.activation(out=G[:, 6, 8 + h:9 + h], in_=pn[:, j * 129 + 128:j * 129 + 129], func=AF.Abs),
                     [pnb], [gOut_b])
            P.op("dve", lambda v: v.tensor_tensor(out=G[:, 7, 0:8], in0=G[:, 6, 8:16], in1=G[:, 6, 0:8], op=ALU.max), [gsm_b, gOut_b], [gOut_b])
            P.op("dve", lambda v: v.reciprocal(out=G[:, 7, 8:16], in_=G[:, 7, 0:8]), [gOut_b], [gOut_b])
            P.op("dve", lambda v: v.memset(G[:, 8, :], 0.0), [], [gOut_b])
            for h in range(8):
                pn, pnb, j = nums[h]
                P.op("act", lambda a, h=h, pn=pn, j=j: a.activation(out=junk[:, :], in_=pn[:, j * 129:j * 129 + 128], func=AF.Square, scale=G[:, 7, 8 + h:9 + h],
                                                                    accum_out=G[:, 8, h:h + 1]), [pnb, gOut_b], [junk_b, gOut_b])
            P.op("act", lambda a: a.activation(out=G[:, 9, 0:8], in_=G[:, 8, 0:8], func=AF.Ln, scale=1.0 / 128, bias=EPS), [gOut_b], [gOut_b])
            P.op("act", lambda a: a.activation(out=G[:, 8, 8:16], in_=G[:, 9, 0:8], func=AF.Exp, scale=-0.5), [gOut_b], [gOut_b])
            P.op("dve", lambda v: v.tensor_tensor(out=G[:, 9, 8:16], in0=G[:, 8, 8:16], in1=G[:, 7, 8:16], op=ALU.mult), [gOut_b], [gOut_b])
            P.op("dve", lambda v: v.tensor_tensor(out=gw[:, :], in0=sotok[:, c, :], in1=hnorm[:, l, :], op=ALU.mult), [sotok_b[c], const_b], [gw_b])
            for h in range(8):
                pn, pnb, j = nums[h]
                P.op("dve", lambda v, h=h, pn=pn, j=j: v.scalar_tensor_tensor(out=hstok[:, h * 128:(h + 1) * 128], in0=pn[:, j * 129:j * 129 + 128],
                                                                               scalar=G[:, 9, 8 + h:9 + h], in1=gw[:, h * 128:(h + 1) * 128], op0=ALU.mult, op1=ALU.mult),
                     [pnb, gOut_b, gw_b], [hstok_b])
            P.transposes([(pst[:, h * 128:(h + 1) * 128], hstok[:, h * 128:(h + 1) * 128], ident[:, :], [hstok_b, ident_b]) for h in range(8)], [pst_b])
            P.op("act", lambda a: a.copy(out=sq[:, :, cs], in_=pst[:, :].rearrange("p (k t) -> p k t", t=128)), [pst_b], sq_bs)
        for half in range(2):
            wdst, w_b = w_get("mout%d_%d" % (l, half * 512))
            proj_fm(wdst, w_b, 0, 4, sq, sq_bs, KC,
                    lambda mi, pt, pb, half=half: evac(mi, lambda a: a.copy(out=yT[:, half * 4 + mi, :], in_=pt[:, :]),
                                                       lambda v: v.tensor_copy(out=yT[:, half * 4 + mi, :], in_=pt[:, :]), [pb], [yT_b]))

    def kv_stage(seq, ti):
        gblk0 = seq * nblk + ti * NCH
        if ti == 0:
            P.op("dve", lambda v: v.memset(ncprev[:], 0.0), [], [ncprev_b])
        prenorm(16)
        P.op("dve", lambda v: v.memset(kTs[64:65, :, :], 1.0), [], [kTs_b])
        for half in range(2):
            wdst, w_b = w_get("kv_%d" % (half * 512))
            proj_fm(wdst, w_b, 0, 8, hT, hT_bs, KC,
                    lambda h, pt, pb, half=half: evac(h, lambda a: a.copy(out=kTs[0:64, half * 8 + h, :], in_=pt[0:64, :]),
                                                      lambda v: v.tensor_copy(out=kTs[0:64, half * 8 + h, :], in_=pt[0:64, :]), [pb], [kTs_b]), mwidth=64)
        for half in range(2):
            wdst, w_b = w_get("kv_%d" % (1024 + half * 512))
            for c in range(NCH):
                pt, pb = ps_r.next()
                P.mm([(pt[:, :], hT[:, kc, c * 128:(c + 1) * 128], wdst[:, kc, :], kc == 0, kc == KC - 1, [w_b, hT_bs[kc]]) for kc in range(KC)], [pb])
                evac(c + half, lambda a: a.copy(out=vts[:, c, half * 8:(half + 1) * 8, 0:64], in_=pt[:, :].rearrange("p (h d) -> p h d", d=64)),
                     lambda v: v.tensor_copy(out=vts[:, c, half * 8:(half + 1) * 8, 0:64], in_=pt[:, :].rearrange("p (h d) -> p h d", d=64)), [pb], [vts_b])
        wf, wf_b = w_get("kvf")
        G = gsm
        for c in range(NCH):
            cs = slice(c * 128, (c + 1) * 128)
            blk = ti * NCH + c
            pg, pgb = ps_r.next()
            P.mm([(pg[:, 0:16], hT[:, kc, cs], wf[:, kc, :], kc == 0, kc == KC - 1, [wf_b, hT_bs[kc]]) for kc in range(KC)], [pgb])
            P.op("dve", lambda v: v.tensor_tensor(out=G[:, 0, :], in0=pg[:, 0:16], in1=kvb[:, :], op=ALU.add), [pgb], [gsm_b])
            P.op("act", lambda a: a.activation(out=G[:, 1, :], in_=G[:, 0, :], func=AF.Exp, scale=-1.0), [gsm_b], [gsm_b])
            P.op("act", lambda a: a.activation(out=G[:, 1, :], in_=G[:, 1, :], func=AF.Ln, bias=1.0), [gsm_b], [gsm_b])
            pc, pcb = ps_r.next()
            P.mm([(pc[:, 0:16], trif[:, :], G[:, 1, :], True, True, [gsm_b]),
                  (pc[:, 16:32], onesf[:, :], G[:, 1, :], True, True, [gsm_b])], [pcb])
            P.op("dve", lambda v: v.tensor_tensor(out=negc[:, blk, :], in0=pc[:, 0:16], in1=ncprev[:, :], op=ALU.add), [pcb, ncprev_b], [negc_b])
            P.op("dve", lambda v: v.tensor_tensor(out=ncprev[:, :], in0=pc[:, 16:32], in1=ncprev[:, :], op=ALU.add), [pcb], [ncprev_b])
        kvbufs = []
        for c in range(NCH):
            kb_ = Buf("kscr")
            vb_ = Buf("vscr")
            P.dma("sp", kscr_d[gblk0 + c], kTs[0:65, :, c * 128:(c + 1) * 128], [kTs_b], [kb_], kvwpool)
            P.dma("sp", vscr_d[gblk0 + c], vts[:, c, :, :], [vts_b], [vb_], kvwpool)
            kvbufs.append((kb_, vb_))
        return kvbufs

    def fox(j, seq, ti, kvb_all):
        for half in range(2):
            wdst, w_b = w_get("qg%d_%d" % (j, half * 512))
            proj_fm(wdst, w_b, 0, 8, hT, hT_bs, KC,
                    lambda h, pt, pb, half=half: evac(h, lambda a: a.mul(out=QTa[0:64, half * 8 + h, :], in_=pt[0:64, :], mul=0.125),
                                                      lambda v: v.tensor_scalar(out=QTa[0:64, half * 8 + h, :], in0=pt[0:64, :], scalar1=0.125, scalar2=None, op0=ALU.mult),
                                                      [pb], [QTa_b]), mwidth=64)
        for c in range(NCH):
            cs = slice(c * 128, (c + 1) * 128)
            blk = ti * NCH + c
            P.op("dve", lambda v: v.tensor_scalar(out=Caug[:, :, 64], in0=negc[:, blk, :], scalar1=-1.0, scalar2=None, op0=ALU.mult), [negc_b], [Caug_b])
            for hg in range(4):
                pr, prb = ps_r.next()
                P.mm([(pr[0:65, jj * 128:(jj + 1) * 128], Caug[:, hg * 4 + jj, :], ident[:, :], True, True, [Caug_b, ident_b]) for jj in range(4)], [prb])
                evac(hg, lambda a, pr=pr, hg=hg: a.copy(out=QTa[64:65, hg * 4:hg * 4 + 4, cs], in_=pr[64:65, :].rearrange("p (j t) -> p j t", t=128)),
                     lambda v, pr=pr, hg=hg: v.tensor_copy(out=QTa[64:65, hg * 4:hg * 4 + 4, cs], in_=pr[64:65, :].rearrange("p (j t) -> p j t", t=128)),
                     [prb], [QTrow_b])
        for half in range(2):
            wdst, w_b = w_get("qg%d_%d" % (j, 1024 + half * 512))
            proj_fm(wdst, w_b, 0, 8, hT, hT_bs, KC,
                    lambda h, pt, pb, half=half: P.op("act", lambda a: a.activation(out=oT[:, half * 8 + h, :], in_=pt[0:64, :], func=AF.Sigmoid), [pb], [oT_bs[half * 8 + h]]),
                    mwidth=64)
        nkb = ti * NCH + NCH
        GB = 8
        kslots = [(qTf[:].rearrange("p a t -> p (a t)").rearrange("p (b h k) -> p b h k", h=4, k=128), [qT_b]),
                  (kTmf[:].rearrange("p a t -> p (a t)").rearrange("p (b h k) -> p b h k", h=4, k=128), [kTm_b])]
        vslots = [(vtok[:].rearrange("p a t -> p (a t)")[:, 0:GB * 4 * 65].rearrange("p (b h d) -> p b h d", h=4, d=65), vtok_b),
                  (sotok[:].rearrange("p a t -> p (a t)")[:, 0:GB * 4 * 65].rearrange("p (b h d) -> p b h d", h=4, d=65), sotok_b)]
        ps_all = list(ps_r.items)
        slot_i = 0
        for hg in range(4):
            accs = ps_all[0:4]
            ps_r.items = ps_all[4:NPS]
            ps_r.i = 0
            for g0 in range(0, nkb, GB):
                nb = min(GB, nkb - g0)
                ksv, ksbs = kslots[slot_i % 2]
                vsv, vsbs = vslots[slot_i % 2]
                slot_i += 1
                gk0 = seq * nblk + g0
                P.dma("sp", ksv[:, 0:nb], kscr_d[gk0:gk0 + nb, :, hg * 4:hg * 4 + 4, :].rearrange("b p h k -> p b h k"),
                      [kvb_all[g0 + i][0] for i in range(nb)], ksbs, stpool)
                P.dma("sp", vsv[:, 0:nb], vscr_d[gk0:gk0 + nb, :, hg * 4:hg * 4 + 4, :].rearrange("b p h d -> p b h d"),
                      [kvb_all[g0 + i][1] for i in range(nb)], vsbs, stpool)
                pairs = [(kb, hh) for kb in range(g0, g0 + nb) for hh in range(4)]

                def emit_S(kb, hh):
                    ksa = ksv[:, kb - g0]
                    jd = kb - ti * NCH
                    q0 = max(jd, 0) * 128
                    h = hg * 4 + hh
                    pS, pSb = ps_r.next()
                    rds = ksbs + [QTa_b]
                    if jd < 0:
                        items = [(pS[:, 0:T], ksa[:, hh, :], QTa[:, h, 0:T], True, True, rds)]
                    else:
                        items = [(pS[:, q0:q0 + 128], ksa[:, hh, :], QTa[:, h, q0:q0 + 128], True, False, rds),
                                 (pS[:, q0:q0 + 128], ident[:, :], negm[:, :], False, True, [ident_b, const_b])]
                        if q0 + 128 < T:
                            items.append((pS[:, q0 + 128:T], ksa[:, hh, :], QTa[:, h, q0 + 128:T], True, True, rds))
                    P.mm(items, [pSb])
                    return pS, pSb, q0

                def emit_rest(kb, hh, pS, pSb, q0):
                    vsa = vsv[:, kb - g0]
                    h = hg * 4 + hh
                    pta, ptb = pT_r.next()
                    P.op("act", lambda a: a.activation(out=pta[:, q0:T], in_=pS[:, q0:T], func=AF.Exp, bias=negc[:, kb, h:h + 1]),
                         [pSb, negc_b], [ptb])
                    pa, pab = accs[hh]
                    P.mm([(pa[0:65, q0:T], vsa[:, hh, :], pta[:, q0:T], kb == 0, kb == nkb - 1, vsbs + [ptb])], [pab])

                cur = emit_S(*pairs[0])
                for pi in range(len(pairs)):
                    nxt = emit_S(*pairs[pi + 1]) if pi + 1 < len(pairs) else None
                    emit_rest(pairs[pi][0], pairs[pi][1], *cur)
                    cur = nxt
            rd4 = [(yc[64:65, hh, :], yc_r.items[hh][1]) for hh in range(4)]
            ns4 = [(ue[0:64, hh, 0:T], ue_r.items[hh][1]) for hh in range(4)]
            for hh in range(4):
                pa, pab = accs[hh]
                P.op("dve", lambda v, pa=pa, hh=hh: v.reciprocal(out=rd4[hh][0], in_=pa[64:65, :]), [pab], [rd4[hh][1]])
            for hh in range(4):
                h = hg * 4 + hh
                pa, pab = accs[hh]
                P.op("dve", lambda v, h=h, pa=pa, hh=hh: v.tensor_tensor(out=ns4[hh][0], in0=pa[0:64, :], in1=oT[:, h, :], op=ALU.mult),
                     [pab, oT_bs[h]], [ns4[hh][1]])
            bcs = {}

            def emit_bc(hh):
                pb_, pbb = ps_r.next()
                P.mm([(pb_[0:64, :], onesf[64:65, 0:64], rd4[hh][0], True, True, [rd4[hh][1]])], [pbb])
                bcs[hh] = (pb_, pbb)

            def emit_m2(hh):
                h = hg * 4 + hh
                pb_, pbb = bcs[hh]
                P.op("dve", lambda v: v.tensor_tensor(out=oT[:, h, :], in0=ns4[hh][0], in1=pb_[0:64, :], op=ALU.mult), [ns4[hh][1], pbb], [oT_bs[h]])

            emit_bc(0); emit_bc(1); emit_bc(2)
            emit_m2(0)
            emit_bc(3)
            emit_m2(1); emit_m2(2); emit_m2(3)
        ps_r.items = ps_all
        ps_r.i = 0
        for q in range(4):
            wdst, w_b = w_get("fout%d_%d" % (j, q * 256))
            proj_fm(wdst, w_b, 0, 2, oT, oT_bs, 16,
                    lambda mi, pt, pb, q=q: evac(mi, lambda a: a.copy(out=yT[:, q * 2 + mi, :], in_=pt[:, :]),
                                                 lambda v: v.tensor_copy(out=yT[:, q * 2 + mi, :], in_=pt[:, :]), [pb], [yT_b]), npart=64)

    out_evs = []
    for seq in range(nseq):
        kvb_all = []
        for ti in range(ntile):
            t0 = seq * seqlen + ti * T
            P.dma("sp", xT[:], xT_d[:, :, t0:t0 + T], [], xT_bs, xpool)
            if do_kv and 1 not in layers:
                kvb_all.extend(kv_stage(seq, ti))
            for l in layers:
                if ti == 0:
                    for m in range(MFF):
                        pass
                    P.op("dve", lambda v, l=l: v.memset(tails[:, l], 0.0), [], [tails_b[l][m] for m in range(MFF)])
                if "m" in parts:
                    prenorm(l * 4 + 0)
                    if l < 2:
                        mlstm(l, ti == 0)
                    else:
                        fox(l - 2, seq, ti, kvb_all)
                    postnorm_residual(l * 4 + 1)
                if "f" in parts:
                    prenorm(l * 4 + 2)
                    ffn(l)
                    postnorm_residual(l * 4 + 3)
                if l == 1 and do_kv:
                    kvb_all.extend(kv_stage(seq, ti))
            out_evs.append(P.dma("sp", outT_d[:, :, t0:t0 + T], xT[:], xT_bs, [], opool))
    if DEBUG == 2:
        dbg = dict(QTa=(QTa, [yT_b], [65, 16, T], BF16), negc=(negc, [negc_b], [128, nblk, 16], F32), oT=(oT, oT_bs, [64, 16, T], BF16),
                   hT=(hT, hT_bs, [128, KC, T], BF16), pT=(pT, [b for _, b in pT_r.items], [128, NPT, T], BF16), rden=(rden, [rden_b], [65, 1, T], F32),
                   numsb=(numsb, [numsb_b], [64, T], F32))
        dpool = P.pool(1)
        for name, (t_, bufs, shape, dt) in dbg.items():
            dd = nc.dram_tensor("dbg_" + name, shape, dt, kind="ExternalOutput").ap()
            out_evs.append(P.dma("sp", dd, t_[:], bufs, [], dpool))
            P.wait("sp", out_evs[-1])
    if DEBUG == 1:
        dbg = dict(qT=(qT, [qT_b], [64, 8, T], BF16), kTm=(kTm, [kTm_b], [64, 8, T], BF16), vtok=(vtok, vtok_b, [128, NCH, D], BF16),
                   sotok=(sotok, sotok_b, [128, NCH, D], BF16), hstok=(hstok, [hstok_b], [128, D], BF16), hsT=(sq, sq_bs, [128, KC, T], BF16),
                   G=(gsm, [gsm_b], [128, 16, 16], F32), Cst=(Cst, Cst_b[0] + Cst_b[1], [64, 2, 8, 129], F32), hT=(hT, hT_bs, [128, KC, T], BF16),
                   yT=(yT, [yT_b], [128, KC, T], F32), ktok=(ktok, ktok_b, [128, NCH, 512], BF16), Est=(Est, Est_b, [128, 2, 8], F32))
        dpool = P.pool(1)
        for name, (t_, bufs, shape, dt) in dbg.items():
            dd = nc.dram_tensor("dbg_" + name, shape, dt, kind="ExternalOutput").ap()
            out_evs.append(P.dma("sp", dd, t_[:], bufs, [], dpool))
            P.wait("sp", out_evs[-1])
    for ev in out_evs[-3:]:
        P.wait("sp", ev)
    assert wstate["pos"] == len(all_specs), (wstate["pos"], len(all_specs))
    build.stats = dict(cnt=dict(P.cnt), nwait=P.nwait)
    build.es = es
    return nc


def _kmajor(w, kc=None):
    K, N = w.shape
    kc = K // 128
    return np.ascontiguousarray(w.reshape(kc, 128, N).transpose(1, 0, 2))


def prep_shared(inp):
    f = np.float32
    sh = {}
    norms = np.zeros((17, D), f)
    for l in range(4):
        norms[l * 4 + 0] = inp["norm_mix_pre"][l]
        norms[l * 4 + 1] = inp["norm_mix_post"][l]
        norms[l * 4 + 2] = inp["norm_ffn_pre"][l]
        norms[l * 4 + 3] = inp["norm_ffn_post"][l]
    norms[16] = inp["kv_norm"]
    sh["norms"] = np.ascontiguousarray(norms.reshape(17, KC, 128).transpose(2, 0, 1))
    for l in range(2):
        sh["w_in%d" % l] = _kmajor(inp["mlstm_w_in"][l])
        sh["w_mout%d" % l] = _kmajor(inp["mlstm_w_out"][l])
        sh["w_qg%d" % l] = _kmajor(inp["fox_w_qg"][l])
        sh["w_fout%d" % l] = np.ascontiguousarray(inp["fox_w_out"][l].reshape(16, 64, D).transpose(1, 0, 2))
    sh["bgate"] = np.ascontiguousarray(np.broadcast_to(inp["mlstm_b_gate"][None], (128, 2, 16))).astype(f)
    sh["hnorm"] = np.ascontiguousarray(np.broadcast_to(inp["mlstm_norm"][None], (128, 2, D))).astype(f)
    sh["kvw"] = _kmajor(inp["kv_w"])
    sh["kvb"] = np.ascontiguousarray(np.broadcast_to(inp["kv_b_f"][None], (128, 16))).astype(f)
    for l in range(4):
        sh["w_up%d" % l] = _kmajor(inp["ffn_w_up"][l])
        sh["w_dn%d" % l] = _kmajor(inp["ffn_w_down"][l])
    cw = np.zeros((4, 4, 2 * DFF), f)
    cw[:, 0:3] = inp["ffn_conv_w"]
    cw[:, 3] = inp["ffn_conv_b"]
    sh["convw"] = np.ascontiguousarray(cw.reshape(4, 4, MFF, 128).transpose(3, 0, 2, 1))
    sh["ident"] = np.eye(128, dtype=f)
    s = np.arange(128)
    sh["tri"] = (s[:, None] <= s[None, :]).astype(f)
    sh["negm"] = np.where(s[:, None] <= s[None, :], 0.0, NEG).astype(f)
    return sh


def x_to_core(xs):
    n, S, _ = xs.shape
    return np.ascontiguousarray(xs.reshape(n * S, KC, 128).transpose(2, 1, 0))


def core_to_x(o, nseq, S):
    return np.ascontiguousarray(o.transpose(2, 1, 0).reshape(nseq, S, D))


def kernel(**inputs):
    inp = {k: np.asarray(v, dtype=np.float32) for k, v in inputs.items()}
    x = inp["x"]
    B, S, _ = x.shape
    ncores = 8
    nseq = B // ncores
    nc = build(nseq, S)
    sh = prep_shared(inp)
    in_maps = []
    for c in range(ncores):
        m = dict(sh)
        m["xT"] = x_to_core(x[c * nseq:(c + 1) * nseq])
        in_maps.append(m)
    res = run_bass_kernel_spmd(nc, in_maps, core_ids=list(range(ncores)))
    out = np.empty_like(x)
    for c in range(ncores):
        out[c * nseq:(c + 1) * nseq] = core_to_x(res.results[c]["outT"], nseq, S)
    return out
```
